# Optimizing a Trainium2 kernel written in Bass

```python
import jax, jax.numpy as jnp
from jax import lax
import numpy as np

D_MODEL = 1024
BATCH = 8
SEQ = 2048
DEPTH = 4

GLA_HEADS = 4
GLA_DK = D_MODEL // 2
GLA_DV = D_MODEL
GLA_HK = GLA_DK // GLA_HEADS
GLA_HV = GLA_DV // GLA_HEADS
GLA_RANK = 16
GLA_GATE_NORM = 16.0
GLA_CHUNK = 64
SWA_HEADS = 16
SWA_KV_HEADS = 2
HEAD_DIM = 64
SWA_GROUP = SWA_HEADS // SWA_KV_HEADS
SWA_DQ = SWA_HEADS * HEAD_DIM
SWA_DKV = SWA_KV_HEADS * HEAD_DIM
WINDOW = 128
ROPE_THETA = 10000.0
N_EXPERTS = 16
N_GROUPS = 4
EXPERTS_PER_GROUP = N_EXPERTS // N_GROUPS
TOP_K = 2
D_EXPERT = 512
N_MOD = 6
EPS = 1e-6

IN_SPLITS = (GLA_DK, GLA_DK, GLA_DV, GLA_DV, GLA_RANK, SWA_DQ, SWA_DKV, SWA_DKV, D_MODEL, D_MODEL)
D_IN = 2 * GLA_DK + 2 * GLA_DV + GLA_RANK + SWA_DQ + 2 * SWA_DKV + 2 * D_MODEL

kernel_name = "hybrid_gla_swa_sinks_grouped_moe_adaln"


def rmsnorm(x, g):
    x32 = x.astype(jnp.float32)
    y = x32 * lax.rsqrt(jnp.mean(x32 * x32, axis=-1, keepdims=True) + EPS)
    return (y * g.astype(jnp.float32)).astype(x.dtype)


def rope(x, pos):
    half = HEAD_DIM // 2
    inv_freq = jnp.power(ROPE_THETA, -jnp.arange(half, dtype=jnp.float32) / half)
    ang = pos.astype(jnp.float32)[..., None] * inv_freq
    cos = jnp.cos(ang)[:, :, None, :]
    sin = jnp.sin(ang)[:, :, None, :]
    x32 = x.astype(jnp.float32)
    x1, x2 = x32[..., :half], x32[..., half:]
    return jnp.concatenate([x1 * cos - x2 * sin, x2 * cos + x1 * sin], axis=-1).astype(x.dtype)


def gla_chunked(q, k, v, g):
    B, S, H, _ = q.shape
    nc = S // GLA_CHUNK

    def to_chunks(t):
        return t.astype(jnp.float32).reshape(B, nc, GLA_CHUNK, H, t.shape[-1]).transpose(1, 0, 3, 2, 4)

    q, k, v, g = to_chunks(q) * GLA_HK ** -0.5, to_chunks(k), to_chunks(v), to_chunks(g)
    b = jnp.cumsum(g, axis=-2)
    b_last = b[..., -1:, :]
    q_e = q * jnp.exp(b)
    k_intra = k * jnp.exp(-b)
    k_state = k * jnp.exp(b_last - b)
    causal = jnp.tril(jnp.ones((GLA_CHUNK, GLA_CHUNK), dtype=bool))
    att = jnp.where(causal, jnp.einsum('nbhtd,nbhsd->nbhts', q_e, k_intra), 0.0)
    o_intra = jnp.einsum('nbhts,nbhsv->nbhtv', att, v)
    decay = jnp.exp(b_last[..., 0, :])

    def step(state, inp):
        q_n, k_n, v_n, d_n = inp
        o_n = jnp.einsum('bhtd,bhdv->bhtv', q_n, state)
        state = d_n[..., None] * state + jnp.einsum('bhsd,bhsv->bhdv', k_n, v_n)
        return state, o_n

    s0 = jnp.zeros((B, H, GLA_HK, GLA_HV), jnp.float32)
    _, o_inter = lax.scan(step, s0, (q_e, k_state, v, decay))
    o = o_intra + o_inter
    return o.transpose(1, 0, 3, 2, 4).reshape(B, S, H, GLA_HV)


def swa_with_sinks(q, k, v, sinks):
    B, S = q.shape[0], q.shape[1]
    nb = S // WINDOW
    qb = q.astype(jnp.float32).reshape(B, nb, WINDOW, SWA_KV_HEADS, SWA_GROUP, HEAD_DIM) * HEAD_DIM ** -0.5
    kb = k.astype(jnp.float32).reshape(B, nb, WINDOW, SWA_KV_HEADS, HEAD_DIM)
    vb = v.astype(jnp.float32).reshape(B, nb, WINDOW, SWA_KV_HEADS, HEAD_DIM)

    def with_prev(t):
        prev = jnp.pad(t[:, :-1], ((0, 0), (1, 0), (0, 0), (0, 0), (0, 0)))
        return jnp.concatenate([prev, t], axis=2)

    kk, vv = with_prev(kb), with_prev(vb)
    s = jnp.einsum('bnqhgd,bnkhd->bhgnqk', qb, kk)
    qi = jnp.arange(WINDOW)[:, None]
    kj = jnp.arange(2 * WINDOW)[None, :]
    diff = WINDOW + qi - kj
    blk = jnp.arange(nb)[:, None, None]
    valid = (diff >= 0) & (diff < WINDOW) & (blk * WINDOW + kj - WINDOW >= 0)
    s = jnp.where(valid, s, -jnp.inf)
    sink = sinks.astype(jnp.float32).reshape(SWA_KV_HEADS, SWA_GROUP)[:, :, None, None, None]
    m = jnp.maximum(s.max(axis=-1, keepdims=True), sink)
    p = jnp.exp(s - m)
    denom = p.sum(axis=-1, keepdims=True) + jnp.exp(sink - m)
    o = jnp.einsum('bhgnqk,bnkhd->bnqhgd', p / denom, vv)
    return o.reshape(B, S, SWA_DQ)


def token_mixer(h, pos, w_in, a_w, a_b, gla_g, sinks, w_pa, w_pb, w_out):
    B, S, _ = h.shape
    split_points = [int(i) for i in np.cumsum(IN_SPLITS)[:-1]]
    q_a, k_a, v_a, r_a, lr_a, q_b, k_b, v_b, gt_a, gt_b = jnp.split(h @ w_in, split_points, axis=-1)

    def heads(t, d):
        return t.reshape(B, S, -1, d)

    g_a = jax.nn.log_sigmoid((lr_a @ a_w + a_b).astype(jnp.float32)) / GLA_GATE_NORM
    o_a = gla_chunked(heads(q_a, GLA_HK), heads(k_a, GLA_HK), heads(v_a, GLA_HV), heads(g_a, GLA_HK))
    o_a = rmsnorm(o_a, gla_g).astype(h.dtype).reshape(B, S, GLA_DV)
    y_a = o_a * jax.nn.silu(r_a)

    qr = rope(heads(q_b, HEAD_DIM), pos)
    kr = rope(heads(k_b, HEAD_DIM), pos)
    y_b = swa_with_sinks(qr, kr, heads(v_b, HEAD_DIM), sinks).astype(h.dtype)

    merged = jax.nn.sigmoid(gt_a) * (y_a @ w_pa) + jax.nn.sigmoid(gt_b) * (y_b @ w_pb)
    return merged @ w_out


def grouped_moe(h, router_w, router_b, w_gate, w_up, w_down):
    B, S, D = h.shape
    t = h.reshape(B * S, D)
    scores = jax.nn.sigmoid((t @ router_w).astype(jnp.float32))
    biased = scores + router_b.astype(jnp.float32)
    grp_scores = lax.top_k(biased.reshape(-1, N_GROUPS, EXPERTS_PER_GROUP), TOP_K)[0].sum(-1)
    grp = jnp.argmax(grp_scores, axis=-1)
    in_grp = (jnp.arange(N_EXPERTS) // EXPERTS_PER_GROUP)[None, :] == grp[:, None]
    _, idx = lax.top_k(jnp.where(in_grp, biased, -jnp.inf), TOP_K)
    w_sel = jnp.take_along_axis(scores, idx, axis=-1)
    w_sel = w_sel / w_sel.sum(axis=-1, keepdims=True)
    combine = jnp.sum(jax.nn.one_hot(idx, N_EXPERTS, dtype=jnp.float32) * w_sel[..., None], axis=1)
    hid = jax.nn.silu(jnp.einsum('nd,edf->nef', t, w_gate)) * jnp.einsum('nd,edf->nef', t, w_up)
    hid = hid * combine[..., None].astype(hid.dtype)
    out = jnp.einsum('nef,efd->nd', hid, w_down)
    return out.reshape(B, S, D)


def setup_inputs(seed: int = 0) -> dict:
    key = jax.random.key(seed)
    ks = jax.random.split(key, 21)
    L, D, E, F = DEPTH, D_MODEL, N_EXPERTS, D_EXPERT
    nrm = jax.random.normal
    return {
        "x": nrm(ks[0], (BATCH, SEQ, D), jnp.float32),
        "c": nrm(ks[1], (BATCH, D), jnp.float32),
        "positions": jnp.broadcast_to(jnp.arange(SEQ, dtype=jnp.int32), (BATCH, SEQ)),
        "ada_w": nrm(ks[2], (L, D, N_MOD * D), jnp.float32) * (0.3 * D ** -0.5),
        "ada_b": nrm(ks[3], (L, N_MOD * D), jnp.float32) * 0.01,
        "norm1_g": 1.0 + 0.05 * nrm(ks[4], (L, D), jnp.float32),
        "norm2_g": 1.0 + 0.05 * nrm(ks[5], (L, D), jnp.float32),
        "final_g": 1.0 + 0.05 * nrm(ks[6], (D,), jnp.float32),
        "w_in": nrm(ks[7], (L, D, D_IN), jnp.float32) * D ** -0.5,
        "gla_alpha_w": nrm(ks[8], (L, GLA_RANK, GLA_DK), jnp.float32) * GLA_RANK ** -0.5,
        "gla_alpha_b": 0.1 * nrm(ks[9], (L, GLA_DK), jnp.float32),
        "gla_norm_g": 1.0 + 0.05 * nrm(ks[10], (L, GLA_HV), jnp.float32),
        "swa_sinks": nrm(ks[11], (L, SWA_HEADS), jnp.float32),
        "w_pa": nrm(ks[12], (L, GLA_DV, D), jnp.float32) * GLA_DV ** -0.5,
        "w_pb": nrm(ks[13], (L, SWA_DQ, D), jnp.float32) * SWA_DQ ** -0.5,
        "w_out": nrm(ks[14], (L, D, D), jnp.float32) * D ** -0.5,
        "router_w": nrm(ks[15], (D, E), jnp.float32) * D ** -0.5,
        "router_b": 0.01 * nrm(ks[16], (E,), jnp.float32),
        "moe_w_gate": nrm(ks[17], (L, E, D, F), jnp.float32) * D ** -0.5,
        "moe_w_up": nrm(ks[18], (L, E, D, F), jnp.float32) * D ** -0.5,
        "moe_w_down": nrm(ks[19], (L, E, F, D), jnp.float32) * F ** -0.5,
    }


def reference(x, c, positions, ada_w, ada_b, norm1_g, norm2_g, final_g, w_in, gla_alpha_w, gla_alpha_b,
              gla_norm_g, swa_sinks, w_pa, w_pb, w_out, router_w, router_b, moe_w_gate, moe_w_up, moe_w_down):
    cond = jax.nn.silu(c)
    for l in range(DEPTH):
        mod = (cond @ ada_w[l] + ada_b[l])[:, None, :]
        sh1, sc1, g1, sh2, sc2, g2 = jnp.split(mod, N_MOD, axis=-1)
        h = rmsnorm(x, norm1_g[l]) * (1.0 + sc1) + sh1
        x = x + g1 * token_mixer(h, positions, w_in[l], gla_alpha_w[l], gla_alpha_b[l], gla_norm_g[l],
                                 swa_sinks[l], w_pa[l], w_pb[l], w_out[l])
        h = rmsnorm(x, norm2_g[l]) * (1.0 + sc2) + sh2
        x = x + g2 * grouped_moe(h, router_w, router_b, moe_w_gate[l], moe_w_up[l], moe_w_down[l])
    return rmsnorm(x, final_g)
```

```python
import numpy as np
from contextlib import ExitStack
import concourse.bass as bass
import concourse.mybir as mybir
from concourse.bass_utils import run_bass_kernel_spmd

F32 = mybir.dt.float32
BF16 = mybir.dt.bfloat16
I32 = mybir.dt.int32
AF = mybir.ActivationFunctionType
ALU = mybir.AluOpType
AX = mybir.AxisListType

D = 1024
KC = 8
S_LEN = 2048
NT = 16
DEPTH = 4
NE = 16
DIN = 6416
C_QA, C_KA, C_VA, C_RA, C_LR, C_QB, C_KB, C_VB, C_GA, C_GB = 0, 512, 1024, 2048, 3072, 3088, 4112, 4240, 4368, 5392
EPS = 1e-6
NSLOT = 3
TWO_PI = 6.283185307179586
MAGIC = 12582912.0
BIG = 1.0e4


class Sched:
    def __init__(self, nc, stack):
        self.nc = nc
        self.stack = stack
        self.ops = []
        self.eng = {"pe": nc.tensor, "act": nc.scalar, "dve": nc.vector,
                    "pool": nc.gpsimd, "sp": nc.sync}
        self.sems = {}

    def add(self, eng, fn, reads=(), writes=()):
        self.ops.append(dict(eng=eng, fn=fn, reads=tuple(reads), writes=tuple(writes),
                             dma=False, sig=False))

    def dma(self, eng, fn, reads=(), writes=(), n=1, key=None):
        if key is None:
            key = "dma:" + str(writes[0])
        self.ops.append(dict(eng=eng, fn=fn, reads=tuple(reads), writes=tuple(writes),
                             dma=True, n=n, key=key, sig=True))

    def _sem(self, name):
        if name not in self.sems:
            nm = "s%d" % len(self.sems)
            self.sems[name] = self.stack.enter_context(self.nc.semaphore(nm))
        return self.sems[name]

    def emit(self):
        ops = self.ops
        last_w = {}
        readers = {}
        for i, op in enumerate(ops):
            deps = {}
            for r in op["reads"]:
                if r in last_w:
                    deps[last_w[r]] = True
            for w in op["writes"]:
                if w in last_w:
                    deps.setdefault(last_w[w], False)
                for rd in readers.get(w, ()):
                    if rd != i:
                        deps.setdefault(rd, False)
            for r in op["reads"]:
                readers.setdefault(r, []).append(i)
            for w in op["writes"]:
                last_w[w] = i
                readers[w] = []
            keep = {}
            for d, raw in deps.items():
                dop = ops[d]
                if not dop["dma"] and dop["eng"] == op["eng"] and not op["dma"]:
                    if op["eng"] == "pe" or not raw:
                        continue
                keep[d] = raw
            op["deps"] = keep
            for d in keep:
                ops[d]["sig"] = True
        cnt = {}
        for op in ops:
            if op["dma"]:
                k = op["key"]
                cnt[k] = cnt.get(k, 0) + 16 * op["n"]
            elif op["sig"]:
                k = "eng:" + op["eng"]
                cnt[k] = cnt.get(k, 0) + 1
            else:
                continue
            op["val"] = cnt[k]
            op["semname"] = k
        waited = {}
        for op in ops:
            e = op["eng"]
            need = {}
            for d in op["deps"]:
                dop = ops[d]
                k = dop["semname"]
                need[k] = max(need.get(k, 0), dop["val"])
            for k, v in need.items():
                if waited.get((e, k), 0) < v:
                    self.eng[e].wait_ge(self._sem(k), v)
                    waited[(e, k)] = v
            if op["dma"]:
                op["fn"](self._sem(op["semname"]))
            else:
                ins = op["fn"]()
                if op["sig"]:
                    ins.then_inc(self._sem(op["semname"]), 1)
        self.cnt = cnt

    def final_wait(self, eng, keys):
        for k in keys:
            self.eng[eng].wait_ge(self._sem(k), self.cnt[k])


def build_program(n_layers=DEPTH, stop_after=None):
    nc = bass.Bass("TRN2", target_bir_lowering=False)

    def din(name, shape, dt=F32):
        return nc.dram_tensor(name, list(shape), dt, kind="ExternalInput").ap()

    x_d = din("x", [S_LEN, D])
    c_d = din("c_col", [128, KC])
    pos_d = din("pos", [128, NT], I32)
    ada_w = din("ada_w", [DEPTH, D, 6 * D])
    ada_b = din("ada_b", [DEPTH, 6 * D])
    n1g = din("norm1_g", [DEPTH, D])
    n2g = din("norm2_g", [DEPTH, D])
    fing = din("final_g", [D])
    w_in = din("w_in", [DEPTH, D, DIN])
    gaw = din("gla_alpha_w", [DEPTH, 16, 512])
    gab = din("gla_alpha_b", [DEPTH, 512])
    glag = din("gla_norm_g", [DEPTH, 256])
    sinks = din("swa_sinks", [DEPTH, 16])
    w_pa = din("w_pa", [DEPTH, D, D])
    w_pb = din("w_pb", [DEPTH, D, D])
    w_out = din("w_out", [DEPTH, D, D])
    rw_d = din("router_w", [D, NE])
    rb_d = din("router_b", [NE])
    wg_d = din("moe_w_gate", [DEPTH, NE, D, 512])
    wu_d = din("moe_w_up", [DEPTH, NE, D, 512])
    wd_d = din("moe_w_down", [DEPTH, NE, 512, D])
    cst_d = din("cst_f32", [128, 3, 128])
    idn_d = din("cst_ident", [128, 128])
    msk_d = din("cst_msk", [128, 2, 128])
    invf_d = din("cst_invf", [128, 32])
    out_d = nc.dram_tensor("out", [S_LEN, D], F32, kind="ExternalOutput").ap()

    with ExitStack() as st:
        S = Sched(nc, st)

        def sb(name, shape, dt):
            return st.enter_context(nc.sbuf_tensor(name, list(shape), dt))

        def pst(name, shape, dt):
            return st.enter_context(nc.psum_tensor(name, list(shape), dt))

        xres = sb("xres", [128, NT, D], F32)
        mod = sb("mod", [128, 3, D], F32)
        wslot = [sb("wslot%d" % i, [128, KC, D], BF16) for i in range(NSLOT)]
        wlr = sb("wlr", [128, KC, 16], BF16)
        wkvb = sb("wkvb", [128, KC, 256], BF16)
        rwb = sb("rwb", [128, KC, NE], BF16)
        aw17 = sb("aw17", [17, 512], BF16)
        lr17 = sb("lr17", [17, 512], BF16)
        bigB = sb("bigB", [128, 4, KC, 512], BF16)
        hT = bigB[:, 0]
        yT = bigB[:, 1]
        mT = bigB[:, 2]
        bq3 = bigB[:, 3].rearrange("p k t -> p (k t)")
        hb_mx = bq3[:, 0:1024]
        junk_mx = bq3[:, 1024:2048]
        vbf = bq3[:, 2048:3072]
        qrT = bq3[:, 3072:4096].rearrange("p (k t) -> p k t", k=KC)
        BQ3 = ["bq3a", "bq3b", "bq3c0", "bq3c1", "bq3d"]
        BQ2 = ["bq2_%d" % j for j in range(KC)]
        cstf = sb("cstf", [128, 3, 128], F32)
        identb = sb("identb", [128, 128], BF16)
        mskb = sb("mskb", [128, 2, 128], BF16)
        invf = sb("invf", [128, 32], F32)
        COS = sb("COS", [128, NT, 32], F32)
        SIN = sb("SIN", [128, NT, 32], F32)
        posi = sb("posi", [128, NT], I32)
        posf = sb("posf", [128, NT], F32)
        ccol = sb("ccol", [128, KC], F32)
        ctmp = sb("ctmp", [128, KC], F32)
        glagb = sb("glagb", [128, 256], F32)
        esink = sb("esink", [128, 16], F32)
        rbb = sb("rbb", [128, NE], F32)
        fA = sb("fA", [128, D], F32)
        fB = sb("fB", [128, D], F32)
        e1 = sb("e1", [128, 512], F32)
        EbT = sb("EbT", [128, 4, 128], F32)
        EnbT = sb("EnbT", [128, 4, 128], F32)
        Ebrev = sb("Ebrev", [128, 512], F32)
        Sf = sb("Sf", [128, 4, 256], F32)
        stat = sb("stat", [128, 64], F32)
        comb = sb("comb", [128, NT, NE], F32)
        rs = [sb("rs%d" % i, [128, NT, 4], F32) for i in range(3)]
        rq = sb("rq", [128, NT], F32)
        ka = sb("ka", [128, 1024], BF16)
        kst = ka[:, 0:512]
        attm = ka[:, 512:1024].rearrange("p (h t) -> p h t", h=4)
        condb = ka[:].rearrange("p (k t) -> p k t", k=KC)
        qeT = sb("qeT", [128, 4, 128], BF16)
        kiT = sb("kiT", [128, 4, 128], BF16)
        SH = sb("SH", [128, 2, 4, 256], BF16)
        Sbf = [SH[:, 0], SH[:, 1]]
        hid = SH[:].rearrange("p a h v -> p (a h v)").rearrange("p (f t) -> p f t", f=4)
        hb_moe = SH[:, 0].rearrange("p h v -> p (h v)")
        junk_moe = SH[:, 1].rearrange("p h v -> p (h v)")
        krd = sb("krd", [128, 2, 2, 64], BF16)
        krT = [sb("krT%d" % i, [128, 2, 128], BF16) for i in range(3)]
        vaug = [sb("vaug%d" % i, [128, 2, 66], BF16) for i in range(3)]
        qrTs = [qrT, vbf.rearrange("p (k t) -> p k t", k=KC)]
        qrTn = [["bq3d"], ["bq3c0", "bq3c1"]]
        sgt = Ebrev
        kvf = EnbT[:].rearrange("p h t -> p (h t)")[:, 0:256]
        Pp = [qeT[:].rearrange("p h t -> p (h t)").rearrange("p (a b q) -> p a b q", a=2, b=2),
              kiT[:].rearrange("p h t -> p (h t)").rearrange("p (a b q) -> p a b q", a=2, b=2)]
        PpN = ["qeT", "kiT"]
        Pp2t = [sb("Pp2_%d" % i, [128, 2, 2, 128], BF16) for i in range(2)]
        PpAll = [[Pp[0], Pp2t[0][:]], [Pp[1], Pp2t[1][:]]]
        PpAllN = [["qeT", "Pp2_0"], ["kiT", "Pp2_1"]]
        EbTf = EbT[:].rearrange("p h t -> p (h t)")
        rt = [e1[:, 0:256].rearrange("p (t e) -> p t e", e=NE), Ebrev[:, 0:256].rearrange("p (t e) -> p t e", e=NE),
              EbTf[:, 0:256].rearrange("p (t e) -> p t e", e=NE)]
        rtN = ["e1", "Ebrev", "EbT"]

        NPB = 7
        pbank = [pst("pb%d" % i, [128, 512], F32) for i in range(NPB)]
        ptr = pst("ptr", [128, KC, 128], BF16)
        pctr = [0]

        def PB():
            i = pctr[0] % NPB
            pctr[0] += 1
            return pbank[i], "pb%d" % i

        wctr = [0]

        def _wl(slot, name, hlf, src, kdim):
            cs = slice(hlf * 512, (hlf + 1) * 512)
            nm = name + "ab"[hlf]
            S.dma("pool", lambda sem: nc.gpsimd.dma_start(out=slot[:, 0:kdim, cs], in_=src).then_inc(sem, 16),
                  writes=[nm], key="dma:" + nm)

        def _wslot():
            i = wctr[0] % NSLOT
            wctr[0] += 1
            return wslot[i], "wslot%d" % i

        def wload(srcs, kdim=KC):
            slot, name = _wslot()
            for hlf in range(2):
                _wl(slot, name, hlf, srcs[hlf], kdim)
            return slot, [name + "a", name + "b"]

        def wload_pair(srcsX, srcsY):
            sx, nx = _wslot()
            sy, ny = _wslot()
            for hlf in range(2):
                _wl(sx, nx, hlf, srcsX[hlf], KC)
                _wl(sy, ny, hlf, srcsY[hlf], KC)
            return sx, [nx + "a", nx + "b"], sy, [ny + "a", ny + "b"]

        def w2(w2d, c0):
            v = w2d.rearrange("(k p) n -> p k n", p=128)
            return [v[:, :, c0:c0 + 512], v[:, :, c0 + 512:c0 + 1024]]

        def wview(w2d, c0, ncol):
            return w2d.rearrange("(k p) n -> p k n", p=128)[:, :, c0:c0 + ncol]

        def act(out, in_, func, reads, writes, bias=0.0, scale=1.0, accum=None):
            if accum is None:
                S.add("act", lambda: nc.scalar.activation(out=out, in_=in_, func=func, bias=bias, scale=scale),
                      reads, writes)
            else:
                S.add("act", lambda: nc.scalar.activation(out=out, in_=in_, func=func, bias=bias, scale=scale,
                                                          accum_out=accum), reads, writes)

        def tt(out, in0, in1, op, reads, writes):
            S.add("dve", lambda: nc.vector.tensor_tensor(out=out, in0=in0, in1=in1, op=op), reads, writes)

        def stt(out, in0, scalar, in1, op0, op1, reads, writes):
            S.add("dve", lambda: nc.vector.scalar_tensor_tensor(out=out, in0=in0, scalar=scalar, in1=in1, op0=op0, op1=op1),
                  reads, writes)

        def ts(out, in0, s1, s2, op0, op1, reads, writes):
            if op1 is None:
                S.add("dve", lambda: nc.vector.tensor_scalar(out=out, in0=in0, scalar1=s1, scalar2=None, op0=op0),
                      reads, writes)
            else:
                S.add("dve", lambda: nc.vector.tensor_scalar(out=out, in0=in0, scalar1=s1, scalar2=s2, op0=op0, op1=op1),
                      reads, writes)

        def cp(out, in_, reads, writes, eng="dve"):
            if eng == "act":
                S.add("act", lambda: nc.scalar.copy(out=out, in_=in_), reads, writes)
            else:
                S.add("dve", lambda: nc.vector.tensor_copy(out=out, in_=in_), reads, writes)

        def memset(ap, val, writes):
            S.add("dve", lambda: nc.vector.memset(ap, val), [], writes)

        def recip(ap, name):
            S.add("dve", lambda: nc.vector.reciprocal(out=ap, in_=ap), [name], [name])

        def reduce(out, in_, op, reads, writes):
            S.add("dve", lambda: nc.vector.tensor_reduce(out=out, in_=in_, axis=AX.X, op=op), reads, writes)

        def mmk(out, lhs_fn, rhs_fn, nk, reads, writes):
            lhs = [lhs_fn(k) for k in range(nk)]
            rhs = [rhs_fn(k) for k in range(nk)]

            def fn():
                for k in range(nk):
                    ins = nc.tensor.matmul(out, lhsT=lhs[k], rhs=rhs[k], start=(k == 0), stop=(k == nk - 1))
                return ins
            S.add("pe", fn, reads, writes)

        def mm1(out, lhsT, rhs, reads, writes):
            S.add("pe", lambda: nc.tensor.matmul(out, lhsT=lhsT, rhs=rhs, start=True, stop=True), reads, writes)

        def sdma(out, in_, reads, writes, key=None):
            S.dma("sp", lambda sem: nc.sync.dma_start(out=out, in_=in_).then_inc(sem, 16), reads, writes, key=key)

        def pdma(out, in_, reads, writes, key=None):
            S.dma("pool", lambda sem: nc.gpsimd.dma_start(out=out, in_=in_).then_inc(sem, 16), reads, writes, key=key)

        def transpose8(src, src_name, dst, dst_names, nch=8, eng="act"):
            def fn():
                for j in range(nch):
                    ins = nc.tensor.transpose(ptr[:, j, :], src[:, j * 128:(j + 1) * 128], identb[:])
                return ins
            srcn = [src_name] if isinstance(src_name, str) else list(src_name)
            S.add("pe", fn, srcn + ["identb"], ["ptr"])
            cp(dst, ptr[:, 0:nch, :], ["ptr"], dst_names, eng=eng)

        def rstd_from(ssq, scale, name):
            act(ssq, ssq, AF.Ln, [name], [name], bias=EPS, scale=scale)
            act(ssq, ssq, AF.Exp, [name], [name], scale=-0.5)

        def sigmoid_into(buf, src, rnames, wname):
            act(buf, src, AF.Exp, rnames, [wname], scale=-1.0)
            act(buf, buf, AF.Ln, [wname], [wname], bias=1.0)
            act(buf, buf, AF.Exp, [wname], [wname], scale=-1.0)

        for q in range(4):
            sdma(xres[:, 4 * q:4 * q + 4, :],
                 x_d[512 * q:512 * (q + 1), :].rearrange("(t p) d -> p t d", p=128), [],
                 ["x%d" % (4 * q + i) for i in range(4)], key="dma:xin%d" % q)
        sdma(cstf[:], cst_d, [], ["cstf"])
        mskf = e1[:, 0:256].rearrange("p (a q) -> p a q", a=2)
        identf = Ebrev[:, 0:128]
        sdma(mskf, msk_d, [], ["e1"], key="dma:mskf")
        sdma(identf, idn_d, [], ["Ebrev"], key="dma:identf")
        sdma(invf[:], invf_d, [], ["invf"])
        sdma(posi[:], pos_d, [], ["posi"])
        sdma(ccol[:], c_d, [], ["ccol"])
        sdma(rbb[:], rb_d.partition_broadcast(128), [], ["rbb"])
        pdma(rwb[:], rw_d.rearrange("(k p) n -> p k n", p=128), [], ["rwb"])
        cp(identb[:], identf, ["Ebrev"], ["identb"])
        cp(mskb[:], mskf, ["e1"], ["mskb"])
        memset(lr17[:], 1.0, ["lr17"])
        for i in range(3):
            memset(vaug[i][:], 1.0, ["vaug%d" % i])
        act(ctmp[:], ccol[:], AF.Exp, ["ccol"], ["ctmp"], scale=-1.0)
        ts(ctmp[:], ctmp[:], 1.0, None, ALU.add, None, ["ctmp"], ["ctmp"])
        recip(ctmp[:], "ctmp")
        tt(ctmp[:], ctmp[:], ccol[:], ALU.mult, ["ctmp", "ccol"], ["ctmp"])
        angt = fA[:, 0:512].rearrange("p (t d) -> p t d", d=32)
        angk = fB[:, 0:512].rearrange("p (t d) -> p t d", d=32)
        cp(posf[:], posi[:], ["posi"], ["posf"])
        tt(angt, posf[:].unsqueeze(2).to_broadcast([128, NT, 32]), invf[:].unsqueeze(1).to_broadcast([128, NT, 32]),
           ALU.mult, ["posf", "invf"], ["fA"])
        for (dst, dname, shift) in ((SIN, "SIN", 0.0), (COS, "COS", TWO_PI / 4)):
            if shift != 0.0:
                ts(angt, angt, shift, None, ALU.add, None, ["fA"], ["fA"])
            ts(angk, angt, 1.0 / TWO_PI, MAGIC, ALU.mult, ALU.add, ["fA"], ["fB"])
            ts(angk, angk, MAGIC, -TWO_PI, ALU.subtract, ALU.mult, ["fB"], ["fB"])
            tt(angk, angk, angt, ALU.add, ["fB", "fA"], ["fB"])
            ts(angk, angk, -3.14159, 3.14159, ALU.max, ALU.min, ["fB"], ["fB"])
            act(dst[:], angk, AF.Sin, ["fB"], [dname])

        def ada_part(l, part):
            cp(condb, ctmp[:].unsqueeze(2).to_broadcast([128, KC, 128]), ["ctmp"], ["kst", "attm"])
            for j in range(3):
                c0 = (3 * part + j) * D
                slot, sname = wload(w2(ada_w[l], c0))
                sdma(mod[:, j, :], ada_b[l, c0:c0 + D].partition_broadcast(128), [], ["mod%d" % j])
                for half in range(2):
                    hs = slice(half * 512, (half + 1) * 512)
                    pb, pn = PB()
                    mmk(pb[:], lambda k: condb[:, k, :], lambda k: slot[:, k, hs], KC, ["kst", "attm", sname[half]], [pn])
                    tt(mod[:, j, hs], pb[:], mod[:, j, hs], ALU.add, [pn, "mod%d" % j], ["mod%d" % j])
            ng = n1g if part == 0 else n2g
            sdma(fB[:], ng[l].partition_broadcast(128), [], ["fB"])
            stt(mod[:, 1, :], mod[:, 1, :], 1.0, fB[:], ALU.add, ALU.mult, ["mod1", "fB"], ["mod1"])

        def norm_stats(tiles, sqj, sqjn):
            n = len(tiles)
            memset(stat[:, 0:n], 0.0, ["stat"])
            for i, t in enumerate(tiles):
                act(sqj, xres[:, t, :], AF.Square, ["x%d" % t], [sqjn, "stat"], accum=stat[:, i:i + 1])
            rstd_from(stat[:, 0:n], 1.0 / D, "stat")

        def norm_apply(i, t, hbs, alt):
            fb, fbn = [(fA, "fA"), (fB, "fB")][(i % 2) if alt else 0]
            hb, hbn = hbs[(i % 2) if alt else 0]
            stt(fb[:], xres[:, t, :], stat[:, i:i + 1], mod[:, 1, :], ALU.mult, ALU.mult,
                ["x%d" % t, "stat", "mod1"], [fbn])
            tt(hb, fb[:], mod[:, 0, :], ALU.add, [fbn, "mod0"], [hbn])
            return hb, hbn

        def norm_tiles(tiles, dsts, dst_names, hbs, sqj, sqjn, alt=True):
            norm_stats(tiles, sqj, sqjn)
            for i, t in enumerate(tiles):
                hb, hbn = norm_apply(i, t, hbs, alt)
                transpose8(hb, hbn, dsts[i], dst_names[i])

        def gla_tile(l, i, t, sQK, nQK, sV, nV, sR, nR):
            ts_ = slice(i * 128, (i + 1) * 128)
            hcol = lambda h: slice(h * 128, (h + 1) * 128)
            EnbTf = EnbT[:].rearrange("p h t -> p (h t)")
            zps, zn = PB()
            mm1(zps[:], lr17[0:17, ts_], aw17[0:17, :], ["lr17", "aw17"], [zn])
            act(e1[:], zps[:], AF.Exp, [zn], ["e1"], scale=-1.0)
            act(e1[:], e1[:], AF.Ln, ["e1"], ["e1"], bias=1.0)
            for half in range(2):
                hs = slice(half * 512, (half + 1) * 512)
                vps, vn = PB()
                mmk(vps[:], lambda k: hT[:, k, ts_], lambda k: sV[:, k, hs], KC, [nV[half], "bq0"], [vn])
                cp(vbf[:, hs], vps[:], [vn], ["bq3c%d" % half], eng="act")
            qps, qn = PB()
            for h in range(4):
                mmk(qps[:, hcol(h)], lambda k: sQK[:, k, hcol(h)], lambda k: hT[:, k, ts_], KC, [nQK[0], "bq0"], [qn])
            kps, kn = PB()
            for h in range(4):
                mmk(kps[:, hcol(h)], lambda k: sQK[:, k, 512 + h * 128:512 + (h + 1) * 128], lambda k: hT[:, k, ts_], KC,
                    [nQK[1], "bq0"], [kn])
            ktp, ktn = PB()
            mmk(ktp[:], lambda k: hT[:, k, ts_], lambda k: sQK[:, k, 512:1024], KC, [nQK[1], "bq0"], [ktn])
            bps, bn = PB()
            for h in range(4):
                mm1(bps[:, hcol(h)], e1[:, hcol(h)], cstf[:, 0, :], ["e1", "cstf"], [bn])
            rps, rn = PB()
            mm1(rps[:], cstf[:, 1, :], e1[:], ["e1", "cstf"], [rn])
            act(EbTf, bps[:], AF.Exp, [bn], ["EbT"])
            act(EnbTf, bps[:], AF.Exp, [bn], ["EnbT"], scale=-1.0)
            act(Ebrev[:], rps[:], AF.Exp, [rn], ["Ebrev"])
            stt(qeT[:].rearrange("p h t -> p (h t)"), qps[:], 128.0 ** -0.5, EbTf, ALU.mult, ALU.mult, [qn, "EbT"], ["qeT"])
            tt(kiT[:].rearrange("p h t -> p (h t)"), kps[:], EnbTf, ALU.mult, [kn, "EnbT"], ["kiT"])
            tt(kst, ktp[:], Ebrev[:], ALU.mult, [ktn, "Ebrev"], ["kst"])
            aps, an = PB()
            for h in range(4):
                mm1(aps[:, hcol(h)], kiT[:, h, :], qeT[:, h, :], ["kiT", "qeT"], [an])
            tt(attm, aps[:].rearrange("p (h t) -> p h t", h=4), cstf[:, 2, :].unsqueeze(1).to_broadcast([128, 4, 128]),
               ALU.mult, [an, "cstf"], ["attm"])

            def dstate(c):
                rows = slice(c * 64, (c + 1) * 64)
                dps = []
                for hp in range(2):
                    dp, dn = PB()
                    dps.append((dp, dn))
                    for hh in range(2):
                        h = 2 * hp + hh
                        mm1(dp[:, hh * 256:(hh + 1) * 256], kst[rows, hcol(h)], vbf[rows, h * 256:(h + 1) * 256],
                            ["kst", "bq3c%d" % (h // 2)], [dn])
                col = 63 if c == 0 else 127
                for h in range(4):
                    dp, dn = dps[h // 2]
                    hh = h % 2
                    stt(Sf[:, h, :], Sf[:, h, :], EbT[:, h, col:col + 1], dp[:, hh * 256:(hh + 1) * 256], ALU.mult, ALU.add,
                        ["Sf", "EbT", dn], ["Sf"])
            dstate(0)
            cp(Sbf[1], Sf[:], ["Sf"], ["Sbf1"], eng="act")
            rpss = []
            for half in range(2):
                hs = slice(half * 512, (half + 1) * 512)
                rp, rpn = PB()
                rpss.append((rp, rpn))
                mmk(rp[:], lambda k: hT[:, k, ts_], lambda k: sR[:, k, hs], KC, [nR[half], "bq0"], [rpn])
                act(fA[:, hs], rp[:], AF.Exp, [rpn], ["fA"], scale=-1.0)
            dstate(1)
            act(fA[:], fA[:], AF.Ln, ["fA"], ["fA"], bias=1.0)
            act(fA[:], fA[:], AF.Exp, ["fA"], ["fA"], scale=-1.0)
            ops_ = []
            for hp in range(2):
                op_, on = PB()
                ops_.append((op_, on))
                for hh in range(2):
                    h = 2 * hp + hh

                    def fn(h=h, hh=hh, op_=op_):
                        cs = slice(hh * 256, (hh + 1) * 256)
                        nc.tensor.matmul(op_[:, cs], lhsT=attm[:, h, :], rhs=vbf[:, h * 256:(h + 1) * 256],
                                         start=True, stop=False)
                        nc.tensor.matmul(op_[0:64, cs], lhsT=qeT[:, h, 0:64], rhs=Sbf[0][:, h, :], start=False, stop=False)
                        return nc.tensor.matmul(op_[64:128, cs], lhsT=qeT[:, h, 64:128], rhs=Sbf[1][:, h, :],
                                                start=False, stop=True)
                    S.add("pe", fn, ["attm", "bq3c%d" % (h // 2), "qeT", "Sbf0", "Sbf1"], [on])
            cp(Sbf[0], Sf[:], ["Sf"], ["Sbf0"], eng="act")
            for half in range(2):
                hs = slice(half * 512, (half + 1) * 512)
                rp, rpn = rpss[half]
                tt(fB[:, hs], rp[:], fA[:, hs], ALU.mult, [rpn, "fA"], ["fB"])
            fB4 = fB[:].rearrange("p (h v) -> p h v", h=4)
            tt(fB4, fB4, glagb[:].unsqueeze(1).to_broadcast([128, 4, 256]), ALU.mult, ["fB", "glagb"], ["fB"])
            memset(stat[:, 8:12], 0.0, ["stat2"])
            for h in range(4):
                op_, on = ops_[h // 2]
                hh = h % 2
                act(junk_mx[:, 0:256], op_[:, hh * 256:(hh + 1) * 256], AF.Square, [on], ["bq3b", "stat2"],
                    accum=stat[:, 8 + h:9 + h])
            rstd_from(stat[:, 8:12], 1.0 / 256, "stat2")
            for h in range(4):
                op_, on = ops_[h // 2]
                hh = h % 2
                stt(hb_mx[:, h * 256:(h + 1) * 256], op_[:, hh * 256:(hh + 1) * 256], stat[:, 8 + h:9 + h],
                    fB[:, h * 256:(h + 1) * 256], ALU.mult, ALU.mult, [on, "stat2", "fB"], ["bq3a"])
            transpose8(hb_mx, "bq3a", yT[:, :, ts_], ["bq1"])

        def gate_merge(sG, nG, sP, nP, accumulate):
            bufs = [(Ebrev, "Ebrev"), (e1, "e1")]
            for j in range(KC):
                js = slice(j * 128, (j + 1) * 128)
                sg, sgn = bufs[j % 2]
                gp, gn = PB()
                mmk(gp[:], lambda k: sG[:, k, js], lambda k: hT[:, k, :], KC, [nG[j // 4], "bq0"], [gn])
                pp, pn = PB()
                mmk(pp[:], lambda k: sP[:, k, js], lambda k: yT[:, k, :], KC, [nP[j // 4], "bq1"], [pn])
                sigmoid_into(sg[:], gp[:], [gn], sgn)
                if not accumulate:
                    tt(mT[:, j, :], pp[:], sg[:], ALU.mult, [pn, sgn], ["bq2_%d" % j])
                else:
                    tt(sg[:], pp[:], sg[:], ALU.mult, [pn, sgn], [sgn])
                    tt(mT[:, j, :], sg[:], mT[:, j, :], ALU.add, [sgn, "bq2_%d" % j], ["bq2_%d" % j])

        def swa_prepA(i, t, sQB, nQB):
            ts_ = slice(i * 128, (i + 1) * 128)
            cur = t % 3
            kp, kpn = PB()
            mmk(kp[:, 0:256], lambda k: hT[:, k, ts_], lambda k: wkvb[:, k, :], KC, ["wkvb", "bq0"], [kpn])
            cp(kvf, kp[:, 0:256], [kpn], ["EnbT"], eng="act")
            for half in range(2):
                hs = slice(half * 512, (half + 1) * 512)
                qp, qn = PB()
                mmk(qp[:], lambda k: hT[:, k, ts_], lambda k: sQB[:, k, hs], KC, [nQB[half], "bq0"], [qn])
                cp(fA[:, hs], qp[:], [qn], ["fA"], eng="act")
            kv4 = kvf[:, 0:128].rearrange("p (h two d) -> p h two d", two=2, d=32)
            k1, k2 = kv4[:, :, 0, :], kv4[:, :, 1, :]
            cos2 = COS[:, t, :].unsqueeze(1).to_broadcast([128, 2, 32])
            sin2 = SIN[:, t, :].unsqueeze(1).to_broadcast([128, 2, 32])
            u1 = rs[0][:].rearrange("p t g -> p (t g)").rearrange("p (h d) -> p h d", d=32)
            kd0 = krd[:, :, 0, :].rearrange("p h (two d) -> p h two d", two=2)
            tt(u1, k2, sin2, ALU.mult, ["EnbT", "SIN"], ["rs0"])
            tt(kd0[:, :, 0, :], k1, cos2, ALU.mult, ["EnbT", "COS"], ["krd"])
            tt(kd0[:, :, 0, :], kd0[:, :, 0, :], u1, ALU.subtract, ["krd", "rs0"], ["krd"])
            tt(u1, k1, sin2, ALU.mult, ["EnbT", "SIN"], ["rs0"])
            tt(kd0[:, :, 1, :], k2, cos2, ALU.mult, ["EnbT", "COS"], ["krd"])
            tt(kd0[:, :, 1, :], kd0[:, :, 1, :], u1, ALU.add, ["krd", "rs0"], ["krd"])
            cp(krd[:, :, 1, :], krd[:, :, 0, :], ["krd"], ["krd"])
            cp(vaug[cur][:, :, 0:64], kvf[:, 128:256].rearrange("p (h d) -> p h d", d=64), ["EnbT"], ["vaug%d" % cur])
            qv = fA[:].rearrange("p (h two d) -> p h two d", two=2, d=32)
            q1, q2 = qv[:, :, 0, :], qv[:, :, 1, :]
            ov = hb_mx.rearrange("p (h two d) -> p h two d", two=2, d=32)
            cosb = COS[:, t, :].unsqueeze(1).to_broadcast([128, 16, 32])
            sinb = SIN[:, t, :].unsqueeze(1).to_broadcast([128, 16, 32])
            t1 = fB[:, 0:512].rearrange("p (h d) -> p h d", d=32)
            t2 = fB[:, 512:1024].rearrange("p (h d) -> p h d", d=32)
            tt(t1, q1, cosb, ALU.mult, ["fA", "COS"], ["fB"])
            tt(t2, q2, sinb, ALU.mult, ["fA", "SIN"], ["fB"])
            tt(ov[:, :, 0, :], t1, t2, ALU.subtract, ["fB"], ["bq3a"])
            tt(t1, q2, cosb, ALU.mult, ["fA", "COS"], ["fB"])
            tt(t2, q1, sinb, ALU.mult, ["fA", "SIN"], ["fB"])
            tt(ov[:, :, 1, :], t1, t2, ALU.add, ["fB"], ["bq3a"])

        def swa_prepB(i, t):
            cur = t % 3

            def fnk():
                for kvh in range(2):
                    ins = nc.tensor.transpose(ptr[:, kvh, :], krd[:, kvh, :, :].rearrange("p a d -> p (a d)"), identb[:])
                return ins
            S.add("pe", fnk, ["krd", "identb"], ["ptr"])
            cp(krT[cur][:], ptr[:, 0:2, :], ["ptr"], ["krT%d" % cur], eng="act")
            transpose8(hb_mx, "bq3a", qrTs[t % 2], qrTn[t % 2])

        def swa_scores(i, t, g):
            cur, prv = t % 3, (t - 1) % 3
            qT_, qTn_ = qrTs[t % 2], qrTn[t % 2]
            kbs = [1] if t == 0 else [0, 1]
            kvh = g // 2
            for par in range(2):
                sp_, sn = PB()
                for kb in kbs:
                    ksrc, ksn = (krT[prv], "krT%d" % prv) if kb == 0 else (krT[cur], "krT%d" % cur)
                    for h2 in range(2):
                        hq = 4 * g + 2 * h2 + par
                        j = hq // 2
                        cs = slice((kb * 2 + h2) * 128, (kb * 2 + h2 + 1) * 128)
                        mm1(sp_[:, cs], ksrc[par * 64:(par + 1) * 64, kvh, :], qT_[par * 64:(par + 1) * 64, j, :],
                            [ksn] + qTn_, [sn])
                lo = 0 if t != 0 else 256
                PP, PPn = PpAll[par][g % 2], PpAllN[par][g % 2]
                Pv = PP.rearrange("p a b q -> p (a b q)")
                act(Pv[:, lo:512], sp_[:, lo:512], AF.Exp, [sn], [PPn], scale=0.125)
                if t != 0:
                    tt(PP, PP, mskb[:].unsqueeze(2).to_broadcast([128, 2, 2, 128]), ALU.mult, [PPn, "mskb"], [PPn])
                else:
                    tt(PP[:, 1, :, :], PP[:, 1, :, :], mskb[:, 1, :].unsqueeze(1).to_broadcast([128, 2, 128]),
                       ALU.mult, [PPn, "mskb"], [PPn])

        def swa_pv(i, t, g):
            cur, prv = t % 3, (t - 1) % 3
            kbs = [1] if t == 0 else [0, 1]
            kvh = g // 2
            op_, on = PB()
            for hh in range(4):
                par, h2 = hh % 2, hh // 2

                def fn(hh=hh, par=par, h2=h2, op_=op_):
                    for n_, kb in enumerate(kbs):
                        vsrc = vaug[prv] if kb == 0 else vaug[cur]
                        ins = nc.tensor.matmul(op_[:, hh * 66:hh * 66 + 65], lhsT=PpAll[par][g % 2][:, kb, h2, :],
                                               rhs=vsrc[:, kvh, 0:65], start=(n_ == 0), stop=(n_ == len(kbs) - 1))
                    return ins
                S.add("pe", fn, [PpAllN[par][g % 2], "vaug%d" % cur, "vaug%d" % prv], [on])
            ov4 = op_[:, 0:264].rearrange("p (h d) -> p h d", d=66)
            dn_ = stat[:, 16 + 4 * (g % 2):20 + 4 * (g % 2)]
            dnn = "stat3_%d" % (g % 2)
            tt(dn_, ov4[:, :, 64], esink[:, 4 * g:4 * g + 4], ALU.add, [on, "esink"], [dnn])
            recip(dn_, dnn)
            tt(junk_mx[:, g * 256:(g + 1) * 256].rearrange("p (h d) -> p h d", d=64), ov4[:, :, 0:64],
               dn_.unsqueeze(2).to_broadcast([128, 4, 64]), ALU.mult, [on, dnn], ["bq3b"])

        def swa_finish(i, t):
            ts_ = slice(i * 128, (i + 1) * 128)
            transpose8(junk_mx, "bq3b", yT[:, :, ts_], ["bq1"])

        def swa_block(tiles, sQB, nQB):
            swa_prepA(0, tiles[0], sQB, nQB)
            swa_prepB(0, tiles[0])
            for i, t in enumerate(tiles):
                swa_scores(i, t, 0)
                swa_scores(i, t, 1)
                swa_pv(i, t, 0)
                if i + 1 < len(tiles):
                    swa_prepA(i + 1, tiles[i + 1], sQB, nQB)
                swa_scores(i, t, 2)
                swa_pv(i, t, 1)
                swa_scores(i, t, 3)
                swa_pv(i, t, 2)
                swa_pv(i, t, 3)
                swa_finish(i, t)
                if i + 1 < len(tiles):
                    swa_prepB(i + 1, tiles[i + 1])

        def out_proj(i, t, sO, nO):
            ts_ = slice(i * 128, (i + 1) * 128)
            for half in range(2):
                hs = slice(half * 512, (half + 1) * 512)
                op_, on = PB()
                mmk(op_[:], lambda k: mT[:, k, ts_], lambda k: sO[:, k, hs], KC, [nO[half]] + BQ2, [on])
                tb_, tbn = [(e1, "e1"), (Ebrev, "Ebrev")][half]
                tt(tb_[:], op_[:], mod[:, 2, hs], ALU.mult, [on, "mod2"], [tbn])
                tt(xres[:, t, hs], xres[:, t, hs], tb_[:], ALU.add, [tbn, "x%d" % t], ["x%d" % t])

        def mixer(l):
            wl = w_in[l]
            pdma(wlr[:], wview(wl, C_LR, 16), [], ["wlr"])

            def ldkv(sem):
                nc.gpsimd.dma_start(out=wkvb[:, :, 0:128], in_=wview(wl, C_KB, 128)).then_inc(sem, 16)
                nc.gpsimd.dma_start(out=wkvb[:, :, 128:256], in_=wview(wl, C_VB, 128)).then_inc(sem, 16)
            S.dma("pool", ldkv, [], ["wkvb"], n=2)

            def ldaw(sem):
                nc.gpsimd.dma_start(out=aw17[0:16, :], in_=gaw[l]).then_inc(sem, 16)
                nc.gpsimd.dma_start(out=aw17[16:17, :], in_=gab[l:l + 1, :]).then_inc(sem, 16)
            S.dma("pool", ldaw, [], ["aw17"], n=2)
            sdma(glagb[:], glag[l].partition_broadcast(128), [], ["glagb"])
            sdma(esink[:], sinks[l].partition_broadcast(128), [], ["esink"])
            act(esink[:], esink[:], AF.Exp, ["esink"], ["esink"])
            memset(Sf[:], 0.0, ["Sf"])
            memset(Sbf[0], 0.0, ["Sbf0"])
            mxhb = [(hb_mx, "bq3a"), (junk_mx, "bq3b")]
            for tb in range(4):
                tiles = [4 * tb + i for i in range(4)]
                if tb == 0:
                    norm_tiles(tiles, [hT[:, :, i * 128:(i + 1) * 128] for i in range(4)], [["bq0"]] * 4,
                               mxhb, vbf, "bq3c0")
                lp, lpn = PB()
                mmk(lp[0:16, :], lambda k: wlr[:, k, :], lambda k: hT[:, k, :], KC, ["wlr", "bq0"], [lpn])
                cp(lr17[0:16, :], lp[0:16, :], [lpn], ["lr17"], eng="act")
                sQK, nQK = wload(w2(wl, C_QA))
                sV, nV = wload(w2(wl, C_VA))
                sR, nR = wload(w2(wl, C_RA))
                for i, t in enumerate(tiles):
                    gla_tile(l, i, t, sQK, nQK, sV, nV, sR, nR)
                sG, nG, sP, nP = wload_pair(w2(wl, C_GA), w2(w_pa[l], 0))
                gate_merge(sG, nG, sP, nP, accumulate=False)
                sQB, nQB = wload(w2(wl, C_QB))
                swa_block(tiles, sQB, nQB)
                sG, nG, sP, nP = wload_pair(w2(wl, C_GB), w2(w_pb[l], 0))
                gate_merge(sG, nG, sP, nP, accumulate=True)
                sO, nO = wload(w2(w_out[l], 0))
                if tb < 3:
                    ntiles = [4 * (tb + 1) + i for i in range(4)]
                    norm_stats(ntiles, vbf, "bq3c0")
                for i, t in enumerate(tiles):
                    if tb < 3:
                        hb, hbn = norm_apply(i, ntiles[i], mxhb, True)
                    out_proj(i, t, sO, nO)
                    if tb < 3:
                        transpose8(hb, hbn, hT[:, :, i * 128:(i + 1) * 128], ["bq0"])

        def moe(l):
            qn_ = [["bq0"], ["bq1"], BQ2, BQ3]
            sc, bi, tm = rt
            scn, bin_, tmn = rtN
            Sff = Sf[:].rearrange("p h v -> p (h v)")
            KA = ["kst", "attm"]
            norm_stats(list(range(NT)), wkvb[:].rearrange("p k n -> p (k n)")[:, 0:1024], "wkvb")

            def napply(tb, i):
                t = 4 * tb + i
                stt(Sff, xres[:, t, :], stat[:, t:t + 1], mod[:, 1, :], ALU.mult, ALU.mult, ["x%d" % t, "stat", "mod1"], ["Sf"])
                tt(ka[:], Sff, mod[:, 0, :], ALU.add, ["Sf", "mod0"], KA)

            def ntrans(tb, i):
                transpose8(ka[:], KA, bigB[:, tb, :, i * 128:(i + 1) * 128], qn_[tb])

            def router(tb):
                tsl = slice(4 * tb, 4 * tb + 4)
                lp, lpn = PB()
                for i in range(4):
                    mmk(lp[:, i * NE:(i + 1) * NE], lambda k: bigB[:, tb, k, i * 128:(i + 1) * 128], lambda k: rwb[:, k, :], KC,
                        ["rwb"] + qn_[tb], [lpn])
                sc_, bi_, tm_ = sc[:, tsl, :], bi[:, tsl, :], tm[:, tsl, :]
                act(sc_.rearrange("p t e -> p (t e)"), lp[:, 0:4 * NE], AF.Tanh, [lpn], [scn], scale=0.5)
                ts(sc_, sc_, 0.5, 0.5, ALU.mult, ALU.add, [scn], [scn])
                tt(bi_, sc_, rbb[:].unsqueeze(1).to_broadcast([128, 4, NE]), ALU.add, [scn, "rbb"], [bin_])
                b4 = bi_.rearrange("p t (g e) -> p t g e", e=4)
                t4 = tm_.rearrange("p t (g e) -> p t g e", e=4)
                m1, m2, gsel, rq_ = rs[0][:, tsl, :], rs[1][:, tsl, :], rs[2][:, tsl, :], rq[:, tsl]
                reduce(m1, b4, ALU.max, [bin_], ["rs0"])
                tt(t4, b4, m1.unsqueeze(3).to_broadcast([128, 4, 4, 4]), ALU.is_equal, [bin_, "rs0"], [tmn])
                stt(tm_, tm_, -BIG, bi_, ALU.mult, ALU.add, [tmn, bin_], [tmn])
                reduce(m2, t4, ALU.max, [tmn], ["rs1"])
                tt(m1, m1, m2, ALU.add, ["rs0", "rs1"], ["rs0"])
                reduce(rq_, m1, ALU.max, ["rs0"], ["rq"])
                tt(gsel, m1, rq_.unsqueeze(2).to_broadcast([128, 4, 4]), ALU.is_equal, ["rs0", "rq"], ["rs2"])
                ts(gsel, gsel, -1.0, BIG, ALU.add, ALU.mult, ["rs2"], ["rs2"])
                tt(t4, b4, gsel.unsqueeze(3).to_broadcast([128, 4, 4, 4]), ALU.add, [bin_, "rs2"], [tmn])
                reduce(rq_, tm_, ALU.max, [tmn], ["rq"])
                tt(bi_, tm_, rq_.unsqueeze(2).to_broadcast([128, 4, NE]), ALU.is_equal, [tmn, "rq"], [bin_])
                stt(tm_, bi_, -BIG, tm_, ALU.mult, ALU.add, [bin_, tmn], [tmn])
                reduce(rq_, tm_, ALU.max, [tmn], ["rq"])
                tt(tm_, tm_, rq_.unsqueeze(2).to_broadcast([128, 4, NE]), ALU.is_equal, [tmn, "rq"], [tmn])
                tt(bi_, bi_, tm_, ALU.add, [bin_, tmn], [bin_])
                tt(bi_, bi_, sc_, ALU.mult, [bin_, scn], [bin_])
                reduce(rq_, bi_, ALU.add, [bin_], ["rq"])
                recip(rq_, "rq")
                tt(comb[:, tsl, :], bi_, rq_.unsqueeze(2).to_broadcast([128, 4, NE]), ALU.mult, [bin_, "rq"], ["comb%d" % tb])

            def gateup(e, tb, sGU, nGU, pre=None, post=None):
                for fc in range(4):
                    if pre is not None:
                        pre(fc)
                    fs = slice(fc * 128, (fc + 1) * 128)
                    gp, gn = PB()
                    mmk(gp[:], lambda k: sGU[:, k, fs], lambda k: bigB[:, tb, k, :], KC, [nGU[0]] + qn_[tb], [gn])
                    up, un = PB()
                    mmk(up[:], lambda k: sGU[:, k, 512 + fc * 128:512 + (fc + 1) * 128], lambda k: bigB[:, tb, k, :], KC,
                        [nGU[1]] + qn_[tb], [un])
                    slb, slname = (fA, "fA") if fc < 2 else (fB, "fB")
                    sls = slb[:, (fc % 2) * 512:(fc % 2 + 1) * 512]
                    act(sls, gp[:], AF.Silu, [gn], [slname])
                    tt(hid[:, fc, :], sls, up[:], ALU.mult, [slname, un], ["Sbf%d" % (fc // 2)])
                    if post is not None:
                        post(fc)

            def down(e, tb, sD, nD):
                for i in range(4):
                    t = 4 * tb + i
                    for half in range(2):
                        hs = slice(half * 512, (half + 1) * 512)
                        op_, on = PB()
                        mmk(op_[:], lambda k: hid[:, k, i * 128:(i + 1) * 128], lambda k: sD[:, k, hs], 4,
                            ["Sbf0", "Sbf1", nD[half]], [on])
                        stt(xres[:, t, hs], op_[:], comb[:, t, e:e + 1], xres[:, t, hs], ALU.mult, ALU.add,
                            [on, "comb%d" % tb, "x%d" % t], ["x%d" % t])

            for i in range(4):
                napply(0, i)
                ntrans(0, i)
            router(0)
            for e in range(NE):
                sGU, nGU = wload([wg_d[l, e].rearrange("(k p) n -> p k n", p=128), wu_d[l, e].rearrange("(k p) n -> p k n", p=128)])
                wdv = wd_d[l, e].rearrange("(k p) n -> p k n", p=128)
                sD, nD = wload([wdv[:, :, 0:512], wdv[:, :, 512:1024]], kdim=4)
                for hlf in range(2):
                    cs = slice(hlf * 512, (hlf + 1) * 512)
                    tt(sD[:, 0:4, cs], sD[:, 0:4, cs], mod[:, 2, cs].unsqueeze(1).to_broadcast([128, 4, 512]), ALU.mult,
                       [nD[hlf], "mod2"], [nD[hlf]])
                for tb in range(4):
                    if e == 0 and tb < 3:
                        gateup(e, tb, sGU, nGU, pre=lambda fc, tb=tb: napply(tb + 1, fc),
                               post=lambda fc, tb=tb: ntrans(tb + 1, fc))
                        router(tb + 1)
                    else:
                        gateup(e, tb, sGU, nGU)
                    down(e, tb, sD, nD)

        for l in range(n_layers):
            ada_part(l, 0)
            mixer(l)
            if l == n_layers - 1 and stop_after == "mixer":
                break
            ada_part(l, 1)
            moe(l)

        sdma(mod[:, 1, :], fing.partition_broadcast(128), [], ["mod1"])
        memset(stat[:, 32:48], 0.0, ["stat4"])
        for t in range(NT):
            act(junk_moe, xres[:, t, :], AF.Square, ["x%d" % t], ["Sbf1", "stat4"], accum=stat[:, 32 + t:33 + t])
        rstd_from(stat[:, 32:48], 1.0 / D, "stat4")
        okeys = []
        for t in range(NT):
            stt(xres[:, t, :], xres[:, t, :], stat[:, 32 + t:33 + t], mod[:, 1, :], ALU.mult, ALU.mult,
                ["x%d" % t, "stat4", "mod1"], ["x%d" % t])
            key = "dma:out%d" % (t % 4)
            sdma(out_d[t * 128:(t + 1) * 128, :], xres[:, t, :], ["x%d" % t], ["out%d" % t], key=key)
            if key not in okeys:
                okeys.append(key)
        S.emit()
        S.final_wait("sp", okeys)
    return nc


def _consts():
    ident = np.eye(128, dtype=np.float32)
    s = np.arange(128)[:, None]
    t = np.arange(128)[None, :]
    same = (s // 64) == (t // 64)
    tri = np.where(same & (s <= t), -1.0 / 16.0, 0.0).astype(np.float32)
    triu = np.where(same & (s > t), -1.0 / 16.0, 0.0).astype(np.float32)
    mask2 = np.where(same & (s <= t), 1.0, 0.0).astype(np.float32)
    cst = np.stack([tri, triu, mask2], axis=1).astype(np.float32)
    mprev = (s > t).astype(np.float32)
    mcur = (s <= t).astype(np.float32)
    msk = np.stack([mprev, mcur], axis=1).astype(np.float32)
    invf = np.power(np.float32(10000.0), -np.arange(32, dtype=np.float32) / np.float32(32)).astype(np.float32)
    invf = np.broadcast_to(invf[None, :], (128, 32)).copy()
    return cst, msk, invf, ident


_PROG_CACHE = {}


def _run(inputs, n_cores=8, n_layers=DEPTH, stop_after=None):
    key = (n_layers, stop_after)
    if key not in _PROG_CACHE:
        _PROG_CACHE[key] = build_program(n_layers, stop_after)
    nc = _PROG_CACHE[key]
    cst, msk, invf, ident = _consts()
    f32 = lambda a: np.ascontiguousarray(np.asarray(a), dtype=np.float32)
    shared = {k: f32(inputs[k]) for k in
              ["ada_w", "ada_b", "norm1_g", "norm2_g", "final_g", "w_in", "gla_alpha_w", "gla_alpha_b", "gla_norm_g",
               "swa_sinks", "w_pa", "w_pb", "w_out", "router_w", "router_b", "moe_w_gate", "moe_w_up", "moe_w_down"]}
    shared["cst_f32"] = cst
    shared["cst_msk"] = msk
    shared["cst_invf"] = invf
    shared["cst_ident"] = ident
    x = f32(inputs["x"])
    c = f32(inputs["c"])
    pos = np.ascontiguousarray(np.asarray(inputs["positions"]), dtype=np.int32)
    in_maps = []
    for b in range(n_cores):
        m = dict(shared)
        m["x"] = np.ascontiguousarray(x[b])
        m["c_col"] = np.ascontiguousarray(c[b].reshape(KC, 128).T)
        m["pos"] = np.ascontiguousarray(pos[b].reshape(NT, 128).T)
        in_maps.append(m)
    res = run_bass_kernel_spmd(nc, in_maps, core_ids=list(range(n_cores)))
    return np.stack([np.asarray(r["out"], dtype=np.float32) for r in res.results], axis=0)


def kernel(**inputs):
    return _run(inputs, n_cores=8, n_layers=DEPTH)
```

```python
import numpy as np
from contextlib import ExitStack
import concourse.bass as bass
import concourse.mybir as mybir
from concourse.bass_utils import run_bass_kernel_spmd

F32 = mybir.dt.float32
BF16 = mybir.dt.bfloat16
I32 = mybir.dt.int32
AF = mybir.ActivationFunctionType
ALU = mybir.AluOpType
AX = mybir.AxisListType

D = 1024
KC = 8
S_LEN = 2048
NT = 16
DEPTH = 4
NE = 16
DIN = 6416
C_QA, C_KA, C_VA, C_RA, C_LR, C_QB, C_KB, C_VB, C_GA, C_GB = 0, 512, 1024, 2048, 3072, 3088, 4112, 4240, 4368, 5392
EPS = 1e-6
NSLOT = 3
TWO_PI = 6.283185307179586
MAGIC = 12582912.0
BIG = 1.0e4


class Sched:
    def __init__(self, nc, stack):
        self.nc = nc
        self.stack = stack
        self.ops = []
        self.eng = {"pe": nc.tensor, "act": nc.scalar, "dve": nc.vector,
                    "pool": nc.gpsimd, "sp": nc.sync}
        self.sems = {}

    def add(self, eng, fn, reads=(), writes=()):
        self.ops.append(dict(eng=eng, fn=fn, reads=tuple(reads), writes=tuple(writes),
                             dma=False, sig=False))

    def dma(self, eng, fn, reads=(), writes=(), n=1, key=None):
        if key is None:
            key = "dma:" + str(writes[0])
        self.ops.append(dict(eng=eng, fn=fn, reads=tuple(reads), writes=tuple(writes),
                             dma=True, n=n, key=key, sig=True))

    def _sem(self, name):
        if name not in self.sems:
            nm = "s%d" % len(self.sems)
            self.sems[name] = self.stack.enter_context(self.nc.semaphore(nm))
        return self.sems[name]

    def emit(self):
        ops = self.ops
        last_w = {}
        readers = {}
        for i, op in enumerate(ops):
            deps = {}
            for r in op["reads"]:
                if r in last_w:
                    deps[last_w[r]] = True
            for w in op["writes"]:
                if w in last_w:
                    deps.setdefault(last_w[w], False)
                for rd in readers.get(w, ()):
                    if rd != i:
                        deps.setdefault(rd, False)
            for r in op["reads"]:
                readers.setdefault(r, []).append(i)
            for w in op["writes"]:
                last_w[w] = i
                readers[w] = []
            keep = {}
            for d, raw in deps.items():
                dop = ops[d]
                if not dop["dma"] and dop["eng"] == op["eng"] and not op["dma"]:
                    if op["eng"] == "pe" or not raw:
                        continue
                keep[d] = raw
            op["deps"] = keep
            for d in keep:
                ops[d]["sig"] = True
        cnt = {}
        for op in ops:
            if op["dma"]:
                k = op["key"]
                cnt[k] = cnt.get(k, 0) + 16 * op["n"]
            elif op["sig"]:
                k = "eng:" + op["eng"]
                cnt[k] = cnt.get(k, 0) + 1
            else:
                continue
            op["val"] = cnt[k]
            op["semname"] = k
        waited = {}
        for op in ops:
            e = op["eng"]
            need = {}
            for d in op["deps"]:
                dop = ops[d]
                k = dop["semname"]
                need[k] = max(need.get(k, 0), dop["val"])
            for k, v in need.items():
                if waited.get((e, k), 0) < v:
                    self.eng[e].wait_ge(self._sem(k), v)
                    waited[(e, k)] = v
            if op["dma"]:
                op["fn"](self._sem(op["semname"]))
            else:
                ins = op["fn"]()
                if op["sig"]:
                    ins.then_inc(self._sem(op["semname"]), 1)
        self.cnt = cnt

    def final_wait(self, eng, keys):
        for k in keys:
            self.eng[eng].wait_ge(self._sem(k), self.cnt[k])


def build_program(n_layers=DEPTH, stop_after=None):
    nc = bass.Bass("TRN2", target_bir_lowering=False)

    def din(name, shape, dt=F32):
        return nc.dram_tensor(name, list(shape), dt, kind="ExternalInput").ap()

    x_d = din("x", [S_LEN, D])
    c_d = din("c_col", [128, KC])
    pos_d = din("pos", [128, NT], I32)
    ada_w = din("ada_w", [DEPTH, D, 6 * D])
    ada_b = din("ada_b", [DEPTH, 6 * D])
    n1g = din("norm1_g", [DEPTH, D])
    n2g = din("norm2_g", [DEPTH, D])
    fing = din("final_g", [D])
    w_in = din("w_in", [DEPTH, D, DIN])
    gaw = din("gla_alpha_w", [DEPTH, 16, 512])
    gab = din("gla_alpha_b", [DEPTH, 512])
    glag = din("gla_norm_g", [DEPTH, 256])
    sinks = din("swa_sinks", [DEPTH, 16])
    w_pa = din("w_pa", [DEPTH, D, D])
    w_pb = din("w_pb", [DEPTH, D, D])
    w_out = din("w_out", [DEPTH, D, D])
    rw_d = din("router_w", [D, NE])
    rb_d = din("router_b", [NE])
    wg_d = din("moe_w_gate", [DEPTH, NE, D, 512])
    wu_d = din("moe_w_up", [DEPTH, NE, D, 512])
    wd_d = din("moe_w_down", [DEPTH, NE, 512, D])
    cst_d = din("cst_f32", [128, 3, 128])
    idn_d = din("cst_ident", [128, 128])
    msk_d = din("cst_msk", [128, 2, 128])
    invf_d = din("cst_invf", [128, 32])
    out_d = nc.dram_tensor("out", [S_LEN, D], F32, kind="ExternalOutput").ap()

    with ExitStack() as st:
        S = Sched(nc, st)

        def sb(name, shape, dt):
            return st.enter_context(nc.sbuf_tensor(name, list(shape), dt))

        def pst(name, shape, dt):
            return st.enter_context(nc.psum_tensor(name, list(shape), dt))

        xres = sb("xres", [128, NT, D], F32)
        mod = sb("mod", [128, 3, D], F32)
        wslot = [sb("wslot%d" % i, [128, KC, D], BF16) for i in range(NSLOT)]
        wlr = sb("wlr", [128, KC, 16], BF16)
        wkvb = sb("wkvb", [128, KC, 256], BF16)
        rwb = sb("rwb", [128, KC, NE], BF16)
        aw17 = sb("aw17", [17, 512], BF16)
        lr17 = sb("lr17", [17, 512], BF16)
        bigB = sb("bigB", [128, 4, KC, 512], BF16)
        hT = bigB[:, 0]
        yT = bigB[:, 1]
        mT = bigB[:, 2]
        bq3 = bigB[:, 3].rearrange("p k t -> p (k t)")
        hb_mx = bq3[:, 0:1024]
        junk_mx = bq3[:, 1024:2048]
        vbf = bq3[:, 2048:3072]
        qrT = bq3[:, 3072:4096].rearrange("p (k t) -> p k t", k=KC)
        BQ3 = ["bq3a", "bq3b", "bq3c0", "bq3c1", "bq3d"]
        BQ2 = ["bq2_%d" % j for j in range(KC)]
        cstf = sb("cstf", [128, 3, 128], F32)
        identb = sb("identb", [128, 128], BF16)
        mskb = sb("mskb", [128, 2, 128], BF16)
        invf = sb("invf", [128, 32], F32)
        COS = sb("COS", [128, NT, 32], F32)
        SIN = sb("SIN", [128, NT, 32], F32)
        posi = sb("posi", [128, NT], I32)
        posf = sb("posf", [128, NT], F32)
        ccol = sb("ccol", [128, KC], F32)
        ctmp = sb("ctmp", [128, KC], F32)
        glagb = sb("glagb", [128, 256], F32)
        esink = sb("esink", [128, 16], F32)
        rbb = sb("rbb", [128, NE], F32)
        fA = sb("fA", [128, D], F32)
        fB = sb("fB", [128, D], F32)
        e1 = sb("e1", [128, 512], F32)
        EbT = sb("EbT", [128, 4, 128], F32)
        EnbT = sb("EnbT", [128, 4, 128], F32)
        Ebrev = sb("Ebrev", [128, 512], F32)
        Sf = sb("Sf", [128, 4, 256], F32)
        stat = sb("stat", [128, 64], F32)
        comb = sb("comb", [128, NT, NE], F32)
        rs = [sb("rs%d" % i, [128, NT, 4], F32) for i in range(3)]
        rq = sb("rq", [128, NT], F32)
        ka = sb("ka", [128, 1024], BF16)
        kst = ka[:, 0:512]
        attm = ka[:, 512:1024].rearrange("p (h t) -> p h t", h=4)
        condb = ka[:].rearrange("p (k t) -> p k t", k=KC)
        qeT = sb("qeT", [128, 4, 128], BF16)
        kiT = sb("kiT", [128, 4, 128], BF16)
        SH = sb("SH", [128, 2, 4, 256], BF16)
        Sbf = [SH[:, 0], SH[:, 1]]
        hid = SH[:].rearrange("p a h v -> p (a h v)").rearrange("p (f t) -> p f t", f=4)
        hb_moe = SH[:, 0].rearrange("p h v -> p (h v)")
        junk_moe = SH[:, 1].rearrange("p h v -> p (h v)")
        krd = sb("krd", [128, 2, 2, 64], BF16)
        krT = [sb("krT%d" % i, [128, 2, 128], BF16) for i in range(3)]
        vaug = [sb("vaug%d" % i, [128, 2, 66], BF16) for i in range(3)]
        qrTs = [qrT, vbf.rearrange("p (k t) -> p k t", k=KC)]
        qrTn = [["bq3d"], ["bq3c0", "bq3c1"]]
        sgt = Ebrev
        kvf = EnbT[:].rearrange("p h t -> p (h t)")[:, 0:256]
        Pp = [qeT[:].rearrange("p h t -> p (h t)").rearrange("p (a b q) -> p a b q", a=2, b=2),
              kiT[:].rearrange("p h t -> p (h t)").rearrange("p (a b q) -> p a b q", a=2, b=2)]
        PpN = ["qeT", "kiT"]
        Pp2t = [sb("Pp2_%d" % i, [128, 2, 2, 128], BF16) for i in range(2)]
        PpAll = [[Pp[0], Pp2t[0][:]], [Pp[1], Pp2t[1][:]]]
        PpAllN = [["qeT", "Pp2_0"], ["kiT", "Pp2_1"]]
        EbTf = EbT[:].rearrange("p h t -> p (h t)")
        rt = [e1[:, 0:256].rearrange("p (t e) -> p t e", e=NE), Ebrev[:, 0:256].rearrange("p (t e) -> p t e", e=NE),
              EbTf[:, 0:256].rearrange("p (t e) -> p t e", e=NE)]
        rtN = ["e1", "Ebrev", "EbT"]

        NPB = 7
        pbank = [pst("pb%d" % i, [128, 512], F32) for i in range(NPB)]
        ptr = pst("ptr", [128, KC, 128], BF16)
        pctr = [0]

        pring = [NPB]

        def PB():
            i = pctr[0] % pring[0]
            pctr[0] += 1
            return pbank[i], "pb%d" % i

        wctr = [0]

        def _wl(slot, name, hlf, src, kdim):
            cs = slice(hlf * 512, (hlf + 1) * 512)
            nm = name + "ab"[hlf]
            S.dma("pool", lambda sem: nc.gpsimd.dma_start(out=slot[:, 0:kdim, cs], in_=src).then_inc(sem, 16),
                  writes=[nm], key="dma:" + nm)

        def _wslot():
            i = wctr[0] % NSLOT
            wctr[0] += 1
            return wslot[i], "wslot%d" % i

        def wload(srcs, kdim=KC):
            slot, name = _wslot()
            for hlf in range(2):
                _wl(slot, name, hlf, srcs[hlf], kdim)
            return slot, [name + "a", name + "b"]

        def wload_pair(srcsX, srcsY):
            sx, nx = _wslot()
            sy, ny = _wslot()
            for hlf in range(2):
                _wl(sx, nx, hlf, srcsX[hlf], KC)
                _wl(sy, ny, hlf, srcsY[hlf], KC)
            return sx, [nx + "a", nx + "b"], sy, [ny + "a", ny + "b"]

        def w2(w2d, c0):
            v = w2d.rearrange("(k p) n -> p k n", p=128)
            return [v[:, :, c0:c0 + 512], v[:, :, c0 + 512:c0 + 1024]]

        def wview(w2d, c0, ncol):
            return w2d.rearrange("(k p) n -> p k n", p=128)[:, :, c0:c0 + ncol]

        def act(out, in_, func, reads, writes, bias=0.0, scale=1.0, accum=None):
            if accum is None:
                S.add("act", lambda: nc.scalar.activation(out=out, in_=in_, func=func, bias=bias, scale=scale),
                      reads, writes)
            else:
                S.add("act", lambda: nc.scalar.activation(out=out, in_=in_, func=func, bias=bias, scale=scale,
                                                          accum_out=accum), reads, writes)

        def tt(out, in0, in1, op, reads, writes):
            S.add("dve", lambda: nc.vector.tensor_tensor(out=out, in0=in0, in1=in1, op=op), reads, writes)

        def stt(out, in0, scalar, in1, op0, op1, reads, writes):
            S.add("dve", lambda: nc.vector.scalar_tensor_tensor(out=out, in0=in0, scalar=scalar, in1=in1, op0=op0, op1=op1),
                  reads, writes)

        def ts(out, in0, s1, s2, op0, op1, reads, writes):
            if op1 is None:
                S.add("dve", lambda: nc.vector.tensor_scalar(out=out, in0=in0, scalar1=s1, scalar2=None, op0=op0),
                      reads, writes)
            else:
                S.add("dve", lambda: nc.vector.tensor_scalar(out=out, in0=in0, scalar1=s1, scalar2=s2, op0=op0, op1=op1),
                      reads, writes)

        def cp(out, in_, reads, writes, eng="dve"):
            if eng == "act":
                S.add("act", lambda: nc.scalar.copy(out=out, in_=in_), reads, writes)
            else:
                S.add("dve", lambda: nc.vector.tensor_copy(out=out, in_=in_), reads, writes)

        def memset(ap, val, writes):
            S.add("dve", lambda: nc.vector.memset(ap, val), [], writes)

        def recip(ap, name):
            S.add("dve", lambda: nc.vector.reciprocal(out=ap, in_=ap), [name], [name])

        def reduce(out, in_, op, reads, writes):
            S.add("dve", lambda: nc.vector.tensor_reduce(out=out, in_=in_, axis=AX.X, op=op), reads, writes)

        def mmk(out, lhs_fn, rhs_fn, nk, reads, writes):
            lhs = [lhs_fn(k) for k in range(nk)]
            rhs = [rhs_fn(k) for k in range(nk)]

            def fn():
                for k in range(nk):
                    ins = nc.tensor.matmul(out, lhsT=lhs[k], rhs=rhs[k], start=(k == 0), stop=(k == nk - 1))
                return ins
            S.add("pe", fn, reads, writes)

        def mm1(out, lhsT, rhs, reads, writes):
            S.add("pe", lambda: nc.tensor.matmul(out, lhsT=lhsT, rhs=rhs, start=True, stop=True), reads, writes)

        def sdma(out, in_, reads, writes, key=None):
            S.dma("sp", lambda sem: nc.sync.dma_start(out=out, in_=in_).then_inc(sem, 16), reads, writes, key=key)

        def pdma(out, in_, reads, writes, key=None):
            S.dma("pool", lambda sem: nc.gpsimd.dma_start(out=out, in_=in_).then_inc(sem, 16), reads, writes, key=key)

        def transpose8(src, src_name, dst, dst_names, nch=8, eng="act"):
            def fn():
                for j in range(nch):
                    ins = nc.tensor.transpose(ptr[:, j, :], src[:, j * 128:(j + 1) * 128], identb[:])
                return ins
            srcn = [src_name] if isinstance(src_name, str) else list(src_name)
            S.add("pe", fn, srcn + ["identb"], ["ptr"])
            cp(dst, ptr[:, 0:nch, :], ["ptr"], dst_names, eng=eng)

        def rstd_from(ssq, scale, name):
            act(ssq, ssq, AF.Ln, [name], [name], bias=EPS, scale=scale)
            act(ssq, ssq, AF.Exp, [name], [name], scale=-0.5)

        def sigmoid_into(buf, src, rnames, wname):
            act(buf, src, AF.Exp, rnames, [wname], scale=-1.0)
            act(buf, buf, AF.Ln, [wname], [wname], bias=1.0)
            act(buf, buf, AF.Exp, [wname], [wname], scale=-1.0)

        for q in range(4):
            sdma(xres[:, 4 * q:4 * q + 4, :],
                 x_d[512 * q:512 * (q + 1), :].rearrange("(t p) d -> p t d", p=128), [],
                 ["x%d" % (4 * q + i) for i in range(4)], key="dma:xin%d" % q)
        sdma(cstf[:], cst_d, [], ["cstf"])
        mskf = e1[:, 0:256].rearrange("p (a q) -> p a q", a=2)
        identf = Ebrev[:, 0:128]
        sdma(mskf, msk_d, [], ["e1"], key="dma:mskf")
        sdma(identf, idn_d, [], ["Ebrev"], key="dma:identf")
        sdma(invf[:], invf_d, [], ["invf"])
        sdma(posi[:], pos_d, [], ["posi"])
        sdma(ccol[:], c_d, [], ["ccol"])
        sdma(rbb[:], rb_d.partition_broadcast(128), [], ["rbb"])
        pdma(rwb[:], rw_d.rearrange("(k p) n -> p k n", p=128), [], ["rwb"])
        cp(identb[:], identf, ["Ebrev"], ["identb"])
        cp(mskb[:], mskf, ["e1"], ["mskb"])
        memset(lr17[:], 1.0, ["lr17"])
        for i in range(3):
            memset(vaug[i][:], 1.0, ["vaug%d" % i])
        act(ctmp[:], ccol[:], AF.Exp, ["ccol"], ["ctmp"], scale=-1.0)
        ts(ctmp[:], ctmp[:], 1.0, None, ALU.add, None, ["ctmp"], ["ctmp"])
        recip(ctmp[:], "ctmp")
        tt(ctmp[:], ctmp[:], ccol[:], ALU.mult, ["ctmp", "ccol"], ["ctmp"])
        angt = fA[:, 0:512].rearrange("p (t d) -> p t d", d=32)
        angk = fB[:, 0:512].rearrange("p (t d) -> p t d", d=32)
        cp(posf[:], posi[:], ["posi"], ["posf"])
        tt(angt, posf[:].unsqueeze(2).to_broadcast([128, NT, 32]), invf[:].unsqueeze(1).to_broadcast([128, NT, 32]),
           ALU.mult, ["posf", "invf"], ["fA"])
        for (dst, dname, shift) in ((SIN, "SIN", 0.0), (COS, "COS", TWO_PI / 4)):
            if shift != 0.0:
                ts(angt, angt, shift, None, ALU.add, None, ["fA"], ["fA"])
            ts(angk, angt, 1.0 / TWO_PI, MAGIC, ALU.mult, ALU.add, ["fA"], ["fB"])
            ts(angk, angk, MAGIC, -TWO_PI, ALU.subtract, ALU.mult, ["fB"], ["fB"])
            tt(angk, angk, angt, ALU.add, ["fB", "fA"], ["fB"])
            ts(angk, angk, -3.14159, 3.14159, ALU.max, ALU.min, ["fB"], ["fB"])
            act(dst[:], angk, AF.Sin, ["fB"], [dname])

        def ada_part(l, part):
            cp(condb, ctmp[:].unsqueeze(2).to_broadcast([128, KC, 128]), ["ctmp"], ["kst", "attm"])
            for j in range(3):
                c0 = (3 * part + j) * D
                slot, sname = wload(w2(ada_w[l], c0))
                sdma(mod[:, j, :], ada_b[l, c0:c0 + D].partition_broadcast(128), [], ["mod%d" % j])
                for half in range(2):
                    hs = slice(half * 512, (half + 1) * 512)
                    pb, pn = PB()
                    mmk(pb[:], lambda k: condb[:, k, :], lambda k: slot[:, k, hs], KC, ["kst", "attm", sname[half]], [pn])
                    tt(mod[:, j, hs], pb[:], mod[:, j, hs], ALU.add, [pn, "mod%d" % j], ["mod%d" % j])
            ng = n1g if part == 0 else n2g
            sdma(fB[:], ng[l].partition_broadcast(128), [], ["fB"])
            stt(mod[:, 1, :], mod[:, 1, :], 1.0, fB[:], ALU.add, ALU.mult, ["mod1", "fB"], ["mod1"])

        def norm_stats(tiles, sqj, sqjn):
            n = len(tiles)
            memset(stat[:, 0:n], 0.0, ["stat"])
            for i, t in enumerate(tiles):
                act(sqj, xres[:, t, :], AF.Square, ["x%d" % t], [sqjn, "stat"], accum=stat[:, i:i + 1])
            rstd_from(stat[:, 0:n], 1.0 / D, "stat")

        def norm_apply(i, t, hbs, alt):
            fb, fbn = [(fA, "fA"), (fB, "fB")][(i % 2) if alt else 0]
            hb, hbn = hbs[(i % 2) if alt else 0]
            stt(fb[:], xres[:, t, :], stat[:, i:i + 1], mod[:, 1, :], ALU.mult, ALU.mult,
                ["x%d" % t, "stat", "mod1"], [fbn])
            tt(hb, fb[:], mod[:, 0, :], ALU.add, [fbn, "mod0"], [hbn])
            return hb, hbn

        def norm_tiles(tiles, dsts, dst_names, hbs, sqj, sqjn, alt=True):
            norm_stats(tiles, sqj, sqjn)
            for i, t in enumerate(tiles):
                hb, hbn = norm_apply(i, t, hbs, alt)
                transpose8(hb, hbn, dsts[i], dst_names[i])

        vbfs = [vbf, bq3[:, 3072:4096]]
        vbfn = [["bq3c0", "bq3c1"], ["bq3d", "bq3d"]]

        def gla_front(i, t, sQK, nQK, sV, nV):
            ts_ = slice(i * 128, (i + 1) * 128)
            hcol = lambda h: slice(h * 128, (h + 1) * 128)
            vb, vbn = vbfs[t % 2], vbfn[t % 2]
            zps, zn = PB()
            mm1(zps[:], lr17[0:17, ts_], aw17[0:17, :], ["lr17", "aw17"], [zn])
            act(e1[:], zps[:], AF.Exp, [zn], ["e1"], scale=-1.0)
            act(e1[:], e1[:], AF.Ln, ["e1"], ["e1"], bias=1.0)
            for half in range(2):
                hs = slice(half * 512, (half + 1) * 512)
                vps, vn = PB()
                mmk(vps[:], lambda k: hT[:, k, ts_], lambda k: sV[:, k, hs], KC, [nV[half], "bq0"], [vn])
                cp(vb[:, hs], vps[:], [vn], [vbn[half]], eng="act")
            qps, qn = pbank[4], "pb4"
            for h in range(4):
                mmk(qps[:, hcol(h)], lambda k: sQK[:, k, hcol(h)], lambda k: hT[:, k, ts_], KC, [nQK[0], "bq0"], [qn])
            kps, kn = pbank[5], "pb5"
            for h in range(4):
                mmk(kps[:, hcol(h)], lambda k: sQK[:, k, 512 + h * 128:512 + (h + 1) * 128], lambda k: hT[:, k, ts_], KC,
                    [nQK[1], "bq0"], [kn])
            ktp, ktn = pbank[6], "pb6"
            mmk(ktp[:], lambda k: hT[:, k, ts_], lambda k: sQK[:, k, 512:1024], KC, [nQK[1], "bq0"], [ktn])
            return dict(q=(qps, qn), k=(kps, kn), kt=(ktp, ktn), vb=vb, vbn=vbn)

        def gla_backA(i, t, F):
            hcol = lambda h: slice(h * 128, (h + 1) * 128)
            EnbTf = EnbT[:].rearrange("p h t -> p (h t)")
            (qps, qn), (kps, kn), (ktp, ktn) = F["q"], F["k"], F["kt"]
            vb, vbn = F["vb"], F["vbn"]
            bps, bn = PB()
            for h in range(4):
                mm1(bps[:, hcol(h)], e1[:, hcol(h)], cstf[:, 0, :], ["e1", "cstf"], [bn])
            rps, rn = PB()
            mm1(rps[:], cstf[:, 1, :], e1[:], ["e1", "cstf"], [rn])
            act(EbTf, bps[:], AF.Exp, [bn], ["EbT"])
            act(EnbTf, bps[:], AF.Exp, [bn], ["EnbT"], scale=-1.0)
            act(Ebrev[:], rps[:], AF.Exp, [rn], ["Ebrev"])
            stt(qeT[:].rearrange("p h t -> p (h t)"), qps[:], 128.0 ** -0.5, EbTf, ALU.mult, ALU.mult, [qn, "EbT"], ["qeT"])
            tt(kiT[:].rearrange("p h t -> p (h t)"), kps[:], EnbTf, ALU.mult, [kn, "EnbT"], ["kiT"])
            tt(kst, ktp[:], Ebrev[:], ALU.mult, [ktn, "Ebrev"], ["kst"])
            aps, an = PB()
            for h in range(4):
                mm1(aps[:, hcol(h)], kiT[:, h, :], qeT[:, h, :], ["kiT", "qeT"], [an])
            tt(attm, aps[:].rearrange("p (h t) -> p h t", h=4), cstf[:, 2, :].unsqueeze(1).to_broadcast([128, 4, 128]),
               ALU.mult, [an, "cstf"], ["attm"])
            gla_dstate(0, vb, vbn)
            cp(Sbf[1], Sf[:], ["Sf"], ["Sbf1"], eng="act")

        def gla_dstate(c, vb, vbn):
            hcol = lambda h: slice(h * 128, (h + 1) * 128)
            rows = slice(c * 64, (c + 1) * 64)
            dps = []
            for hp in range(2):
                dp, dn = PB()
                dps.append((dp, dn))
                for hh in range(2):
                    h = 2 * hp + hh
                    mm1(dp[:, hh * 256:(hh + 1) * 256], kst[rows, hcol(h)], vb[rows, h * 256:(h + 1) * 256],
                        ["kst", vbn[h // 2]], [dn])
            col = 63 if c == 0 else 127
            for h in range(4):
                dp, dn = dps[h // 2]
                hh = h % 2
                stt(Sf[:, h, :], Sf[:, h, :], EbT[:, h, col:col + 1], dp[:, hh * 256:(hh + 1) * 256], ALU.mult, ALU.add,
                    ["Sf", "EbT", dn], ["Sf"])

        def gla_backB(i, t, F, sR, nR):
            ts_ = slice(i * 128, (i + 1) * 128)
            vb, vbn = F["vb"], F["vbn"]
            rpss = []
            for half in range(2):
                hs = slice(half * 512, (half + 1) * 512)
                rp, rpn = PB()
                rpss.append((rp, rpn))
                mmk(rp[:], lambda k: hT[:, k, ts_], lambda k: sR[:, k, hs], KC, [nR[half], "bq0"], [rpn])
                act(fA[:, hs], rp[:], AF.Exp, [rpn], ["fA"], scale=-1.0)
                cp(fB[:, hs], rp[:], [rpn], ["fB"], eng="act")
            gla_dstate(1, vb, vbn)
            act(fA[:], fA[:], AF.Ln, ["fA"], ["fA"], bias=1.0)
            act(fA[:], fA[:], AF.Exp, ["fA"], ["fA"], scale=-1.0)
            ops_ = []
            for hp in range(2):
                op_, on = PB()
                ops_.append((op_, on))
                for hh in range(2):
                    h = 2 * hp + hh

                    def fn(h=h, hh=hh, op_=op_):
                        cs = slice(hh * 256, (hh + 1) * 256)
                        nc.tensor.matmul(op_[:, cs], lhsT=attm[:, h, :], rhs=vb[:, h * 256:(h + 1) * 256],
                                         start=True, stop=False)
                        nc.tensor.matmul(op_[0:64, cs], lhsT=qeT[:, h, 0:64], rhs=Sbf[0][:, h, :], start=False, stop=True)
                        return nc.tensor.matmul(op_[64:128, cs], lhsT=qeT[:, h, 64:128], rhs=Sbf[1][:, h, :],
                                                start=False, stop=True)
                    S.add("pe", fn, ["attm", vbn[h // 2], "qeT", "Sbf0", "Sbf1"], [on])
            cp(Sbf[0], Sf[:], ["Sf"], ["Sbf0"], eng="act")
            tt(fB[:], fB[:], fA[:], ALU.mult, ["fB", "fA"], ["fB"])
            fB4 = fB[:].rearrange("p (h v) -> p h v", h=4)
            tt(fB4, fB4, glagb[:].unsqueeze(1).to_broadcast([128, 4, 256]), ALU.mult, ["fB", "glagb"], ["fB"])
            memset(stat[:, 8:12], 0.0, ["stat2"])
            for h in range(4):
                op_, on = ops_[h // 2]
                hh = h % 2
                act(junk_mx[:, 0:256], op_[:, hh * 256:(hh + 1) * 256], AF.Square, [on], ["bq3b", "stat2"],
                    accum=stat[:, 8 + h:9 + h])
            rstd_from(stat[:, 8:12], 1.0 / 256, "stat2")
            for h in range(4):
                op_, on = ops_[h // 2]
                hh = h % 2
                stt(hb_mx[:, h * 256:(h + 1) * 256], op_[:, hh * 256:(hh + 1) * 256], stat[:, 8 + h:9 + h],
                    fB[:, h * 256:(h + 1) * 256], ALU.mult, ALU.mult, [on, "stat2", "fB"], ["bq3a"])
            transpose8(hb_mx, "bq3a", yT[:, :, ts_], ["bq1"])

        def gla_block(tiles, sQK, nQK, sV, nV, sR, nR):
            pring[0] = 4
            F = gla_front(0, tiles[0], sQK, nQK, sV, nV)
            for i, t in enumerate(tiles):
                gla_backA(i, t, F)
                Fn = gla_front(i + 1, tiles[i + 1], sQK, nQK, sV, nV) if i + 1 < len(tiles) else None
                gla_backB(i, t, F, sR, nR)
                F = Fn
            pring[0] = NPB

        def gate_merge(sG, nG, sP, nP, accumulate):
            bufs = [(Ebrev, "Ebrev"), (e1, "e1")]
            for j in range(KC):
                js = slice(j * 128, (j + 1) * 128)
                sg, sgn = bufs[j % 2]
                gp, gn = PB()
                mmk(gp[:], lambda k: sG[:, k, js], lambda k: hT[:, k, :], KC, [nG[j // 4], "bq0"], [gn])
                pp, pn = PB()
                mmk(pp[:], lambda k: sP[:, k, js], lambda k: yT[:, k, :], KC, [nP[j // 4], "bq1"], [pn])
                sigmoid_into(sg[:], gp[:], [gn], sgn)
                if not accumulate:
                    tt(mT[:, j, :], pp[:], sg[:], ALU.mult, [pn, sgn], ["bq2_%d" % j])
                else:
                    tt(sg[:], pp[:], sg[:], ALU.mult, [pn, sgn], [sgn])
                    tt(mT[:, j, :], sg[:], mT[:, j, :], ALU.add, [sgn, "bq2_%d" % j], ["bq2_%d" % j])

        def swa_prepA(i, t, sQB, nQB):
            ts_ = slice(i * 128, (i + 1) * 128)
            cur = t % 3
            kp, kpn = PB()
            mmk(kp[:, 0:256], lambda k: hT[:, k, ts_], lambda k: wkvb[:, k, :], KC, ["wkvb", "bq0"], [kpn])
            cp(kvf, kp[:, 0:256], [kpn], ["EnbT"], eng="act")
            for half in range(2):
                hs = slice(half * 512, (half + 1) * 512)
                qp, qn = PB()
                mmk(qp[:], lambda k: hT[:, k, ts_], lambda k: sQB[:, k, hs], KC, [nQB[half], "bq0"], [qn])
                cp(fA[:, hs], qp[:], [qn], ["fA"], eng="act")
            kv4 = kvf[:, 0:128].rearrange("p (h two d) -> p h two d", two=2, d=32)
            k1, k2 = kv4[:, :, 0, :], kv4[:, :, 1, :]
            cos2 = COS[:, t, :].unsqueeze(1).to_broadcast([128, 2, 32])
            sin2 = SIN[:, t, :].unsqueeze(1).to_broadcast([128, 2, 32])
            u1 = rs[0][:].rearrange("p t g -> p (t g)").rearrange("p (h d) -> p h d", d=32)
            kd0 = krd[:, :, 0, :].rearrange("p h (two d) -> p h two d", two=2)
            tt(u1, k2, sin2, ALU.mult, ["EnbT", "SIN"], ["rs0"])
            tt(kd0[:, :, 0, :], k1, cos2, ALU.mult, ["EnbT", "COS"], ["krd"])
            tt(kd0[:, :, 0, :], kd0[:, :, 0, :], u1, ALU.subtract, ["krd", "rs0"], ["krd"])
            tt(u1, k1, sin2, ALU.mult, ["EnbT", "SIN"], ["rs0"])
            tt(kd0[:, :, 1, :], k2, cos2, ALU.mult, ["EnbT", "COS"], ["krd"])
            tt(kd0[:, :, 1, :], kd0[:, :, 1, :], u1, ALU.add, ["krd", "rs0"], ["krd"])
            cp(krd[:, :, 1, :], krd[:, :, 0, :], ["krd"], ["krd"])
            cp(vaug[cur][:, :, 0:64], kvf[:, 128:256].rearrange("p (h d) -> p h d", d=64), ["EnbT"], ["vaug%d" % cur])
            qv = fA[:].rearrange("p (h two d) -> p h two d", two=2, d=32)
            q1, q2 = qv[:, :, 0, :], qv[:, :, 1, :]
            ov = hb_mx.rearrange("p (h two d) -> p h two d", two=2, d=32)
            cosb = COS[:, t, :].unsqueeze(1).to_broadcast([128, 16, 32])
            sinb = SIN[:, t, :].unsqueeze(1).to_broadcast([128, 16, 32])
            t1 = fB[:, 0:512].rearrange("p (h d) -> p h d", d=32)
            t2 = fB[:, 512:1024].rearrange("p (h d) -> p h d", d=32)
            tt(t1, q1, cosb, ALU.mult, ["fA", "COS"], ["fB"])
            tt(t2, q2, sinb, ALU.mult, ["fA", "SIN"], ["fB"])
            tt(ov[:, :, 0, :], t1, t2, ALU.subtract, ["fB"], ["bq3a"])
            tt(t1, q2, cosb, ALU.mult, ["fA", "COS"], ["fB"])
            tt(t2, q1, sinb, ALU.mult, ["fA", "SIN"], ["fB"])
            tt(ov[:, :, 1, :], t1, t2, ALU.add, ["fB"], ["bq3a"])

        def swa_prepB(i, t):
            cur = t % 3

            def fnk():
                for kvh in range(2):
                    ins = nc.tensor.transpose(ptr[:, kvh, :], krd[:, kvh, :, :].rearrange("p a d -> p (a d)"), identb[:])
                return ins
            S.add("pe", fnk, ["krd", "identb"], ["ptr"])
            cp(krT[cur][:], ptr[:, 0:2, :], ["ptr"], ["krT%d" % cur], eng="act")
            transpose8(hb_mx, "bq3a", qrTs[t % 2], qrTn[t % 2])

        def swa_scores(i, t, g):
            cur, prv = t % 3, (t - 1) % 3
            qT_, qTn_ = qrTs[t % 2], qrTn[t % 2]
            kbs = [1] if t == 0 else [0, 1]
            kvh = g // 2
            for par in range(2):
                sp_, sn = PB()
                for kb in kbs:
                    ksrc, ksn = (krT[prv], "krT%d" % prv) if kb == 0 else (krT[cur], "krT%d" % cur)
                    for h2 in range(2):
                        hq = 4 * g + 2 * h2 + par
                        j = hq // 2
                        cs = slice((kb * 2 + h2) * 128, (kb * 2 + h2 + 1) * 128)
                        mm1(sp_[:, cs], ksrc[par * 64:(par + 1) * 64, kvh, :], qT_[par * 64:(par + 1) * 64, j, :],
                            [ksn] + qTn_, [sn])
                lo = 0 if t != 0 else 256
                PP, PPn = PpAll[par][g % 2], PpAllN[par][g % 2]
                Pv = PP.rearrange("p a b q -> p (a b q)")
                act(Pv[:, lo:512], sp_[:, lo:512], AF.Exp, [sn], [PPn], scale=0.125)
                if t != 0:
                    tt(PP, PP, mskb[:].unsqueeze(2).to_broadcast([128, 2, 2, 128]), ALU.mult, [PPn, "mskb"], [PPn])
                else:
                    tt(PP[:, 1, :, :], PP[:, 1, :, :], mskb[:, 1, :].unsqueeze(1).to_broadcast([128, 2, 128]),
                       ALU.mult, [PPn, "mskb"], [PPn])

        def swa_pv(i, t, g):
            cur, prv = t % 3, (t - 1) % 3
            kbs = [1] if t == 0 else [0, 1]
            kvh = g // 2
            op_, on = PB()
            for hh in range(4):
                par, h2 = hh % 2, hh // 2

                def fn(hh=hh, par=par, h2=h2, op_=op_):
                    for n_, kb in enumerate(kbs):
                        vsrc = vaug[prv] if kb == 0 else vaug[cur]
                        ins = nc.tensor.matmul(op_[:, hh * 66:hh * 66 + 65], lhsT=PpAll[par][g % 2][:, kb, h2, :],
                                               rhs=vsrc[:, kvh, 0:65], start=(n_ == 0), stop=(n_ == len(kbs) - 1))
                    return ins
                S.add("pe", fn, [PpAllN[par][g % 2], "vaug%d" % cur, "vaug%d" % prv], [on])
            ov4 = op_[:, 0:264].rearrange("p (h d) -> p h d", d=66)
            dn_ = stat[:, 16 + 4 * (g % 2):20 + 4 * (g % 2)]
            dnn = "stat3_%d" % (g % 2)
            tt(dn_, ov4[:, :, 64], esink[:, 4 * g:4 * g + 4], ALU.add, [on, "esink"], [dnn])
            recip(dn_, dnn)
            tt(junk_mx[:, g * 256:(g + 1) * 256].rearrange("p (h d) -> p h d", d=64), ov4[:, :, 0:64],
               dn_.unsqueeze(2).to_broadcast([128, 4, 64]), ALU.mult, [on, dnn], ["bq3b"])

        def swa_finish(i, t):
            ts_ = slice(i * 128, (i + 1) * 128)
            transpose8(junk_mx, "bq3b", yT[:, :, ts_], ["bq1"])

        def swa_block(tiles, sQB, nQB):
            swa_prepA(0, tiles[0], sQB, nQB)
            swa_prepB(0, tiles[0])
            for i, t in enumerate(tiles):
                swa_scores(i, t, 0)
                swa_scores(i, t, 1)
                swa_pv(i, t, 0)
                if i + 1 < len(tiles):
                    swa_prepA(i + 1, tiles[i + 1], sQB, nQB)
                swa_scores(i, t, 2)
                swa_pv(i, t, 1)
                swa_scores(i, t, 3)
                swa_pv(i, t, 2)
                swa_pv(i, t, 3)
                swa_finish(i, t)
                if i + 1 < len(tiles):
                    swa_prepB(i + 1, tiles[i + 1])

        def out_proj(i, t, sO, nO):
            ts_ = slice(i * 128, (i + 1) * 128)
            for half in range(2):
                hs = slice(half * 512, (half + 1) * 512)
                op_, on = PB()
                mmk(op_[:], lambda k: mT[:, k, ts_], lambda k: sO[:, k, hs], KC, [nO[half]] + BQ2, [on])
                tb_, tbn = [(e1, "e1"), (Ebrev, "Ebrev")][half]
                tt(tb_[:], op_[:], mod[:, 2, hs], ALU.mult, [on, "mod2"], [tbn])
                tt(xres[:, t, hs], xres[:, t, hs], tb_[:], ALU.add, [tbn, "x%d" % t], ["x%d" % t])

        def mixer(l):
            wl = w_in[l]
            pdma(wlr[:], wview(wl, C_LR, 16), [], ["wlr"])

            def ldkv(sem):
                nc.gpsimd.dma_start(out=wkvb[:, :, 0:128], in_=wview(wl, C_KB, 128)).then_inc(sem, 16)
                nc.gpsimd.dma_start(out=wkvb[:, :, 128:256], in_=wview(wl, C_VB, 128)).then_inc(sem, 16)
            S.dma("pool", ldkv, [], ["wkvb"], n=2)

            def ldaw(sem):
                nc.gpsimd.dma_start(out=aw17[0:16, :], in_=gaw[l]).then_inc(sem, 16)
                nc.gpsimd.dma_start(out=aw17[16:17, :], in_=gab[l:l + 1, :]).then_inc(sem, 16)
            S.dma("pool", ldaw, [], ["aw17"], n=2)
            sdma(glagb[:], glag[l].partition_broadcast(128), [], ["glagb"])
            sdma(esink[:], sinks[l].partition_broadcast(128), [], ["esink"])
            act(esink[:], esink[:], AF.Exp, ["esink"], ["esink"])
            memset(Sf[:], 0.0, ["Sf"])
            memset(Sbf[0], 0.0, ["Sbf0"])
            mxhb = [(hb_mx, "bq3a"), (junk_mx, "bq3b")]
            for tb in range(4):
                tiles = [4 * tb + i for i in range(4)]
                if tb == 0:
                    norm_tiles(tiles, [hT[:, :, i * 128:(i + 1) * 128] for i in range(4)], [["bq0"]] * 4,
                               mxhb, vbf, "bq3c0")
                lp, lpn = PB()
                mmk(lp[0:16, :], lambda k: wlr[:, k, :], lambda k: hT[:, k, :], KC, ["wlr", "bq0"], [lpn])
                cp(lr17[0:16, :], lp[0:16, :], [lpn], ["lr17"], eng="act")
                sQK, nQK = wload(w2(wl, C_QA))
                sV, nV = wload(w2(wl, C_VA))
                sR, nR = wload(w2(wl, C_RA))
                gla_block(tiles, sQK, nQK, sV, nV, sR, nR)
                sG, nG, sP, nP = wload_pair(w2(wl, C_GA), w2(w_pa[l], 0))
                gate_merge(sG, nG, sP, nP, accumulate=False)
                sQB, nQB = wload(w2(wl, C_QB))
                swa_block(tiles, sQB, nQB)
                sG, nG, sP, nP = wload_pair(w2(wl, C_GB), w2(w_pb[l], 0))
                gate_merge(sG, nG, sP, nP, accumulate=True)
                sO, nO = wload(w2(w_out[l], 0))
                if tb < 3:
                    ntiles = [4 * (tb + 1) + i for i in range(4)]
                    norm_stats(ntiles, vbf, "bq3c0")
                for i, t in enumerate(tiles):
                    if tb < 3:
                        hb, hbn = norm_apply(i, ntiles[i], mxhb, True)
                    out_proj(i, t, sO, nO)
                    if tb < 3:
                        transpose8(hb, hbn, hT[:, :, i * 128:(i + 1) * 128], ["bq0"])

        def moe(l):
            qn_ = [["bq0"], ["bq1"], BQ2, BQ3]
            sc, bi, tm = rt
            scn, bin_, tmn = rtN
            Sff = Sf[:].rearrange("p h v -> p (h v)")
            KA = ["kst", "attm"]
            norm_stats(list(range(NT)), wkvb[:].rearrange("p k n -> p (k n)")[:, 0:1024], "wkvb")

            def napply(tb, i):
                t = 4 * tb + i
                stt(Sff, xres[:, t, :], stat[:, t:t + 1], mod[:, 1, :], ALU.mult, ALU.mult, ["x%d" % t, "stat", "mod1"], ["Sf"])
                tt(ka[:], Sff, mod[:, 0, :], ALU.add, ["Sf", "mod0"], KA)

            def ntrans(tb, i):
                transpose8(ka[:], KA, bigB[:, tb, :, i * 128:(i + 1) * 128], qn_[tb])

            def router(tb):
                tsl = slice(4 * tb, 4 * tb + 4)
                lp, lpn = PB()
                for i in range(4):
                    mmk(lp[:, i * NE:(i + 1) * NE], lambda k: bigB[:, tb, k, i * 128:(i + 1) * 128], lambda k: rwb[:, k, :], KC,
                        ["rwb"] + qn_[tb], [lpn])
                sc_, bi_, tm_ = sc[:, tsl, :], bi[:, tsl, :], tm[:, tsl, :]
                act(sc_.rearrange("p t e -> p (t e)"), lp[:, 0:4 * NE], AF.Tanh, [lpn], [scn], scale=0.5)
                ts(sc_, sc_, 0.5, 0.5, ALU.mult, ALU.add, [scn], [scn])
                tt(bi_, sc_, rbb[:].unsqueeze(1).to_broadcast([128, 4, NE]), ALU.add, [scn, "rbb"], [bin_])
                b4 = bi_.rearrange("p t (g e) -> p t g e", e=4)
                t4 = tm_.rearrange("p t (g e) -> p t g e", e=4)
                m1, m2, gsel, rq_ = rs[0][:, tsl, :], rs[1][:, tsl, :], rs[2][:, tsl, :], rq[:, tsl]
                reduce(m1, b4, ALU.max, [bin_], ["rs0"])
                tt(t4, b4, m1.unsqueeze(3).to_broadcast([128, 4, 4, 4]), ALU.is_equal, [bin_, "rs0"], [tmn])
                stt(tm_, tm_, -BIG, bi_, ALU.mult, ALU.add, [tmn, bin_], [tmn])
                reduce(m2, t4, ALU.max, [tmn], ["rs1"])
                tt(m1, m1, m2, ALU.add, ["rs0", "rs1"], ["rs0"])
                reduce(rq_, m1, ALU.max, ["rs0"], ["rq"])
                tt(gsel, m1, rq_.unsqueeze(2).to_broadcast([128, 4, 4]), ALU.is_equal, ["rs0", "rq"], ["rs2"])
                ts(gsel, gsel, -1.0, BIG, ALU.add, ALU.mult, ["rs2"], ["rs2"])
                tt(t4, b4, gsel.unsqueeze(3).to_broadcast([128, 4, 4, 4]), ALU.add, [bin_, "rs2"], [tmn])
                reduce(rq_, tm_, ALU.max, [tmn], ["rq"])
                tt(bi_, tm_, rq_.unsqueeze(2).to_broadcast([128, 4, NE]), ALU.is_equal, [tmn, "rq"], [bin_])
                stt(tm_, bi_, -BIG, tm_, ALU.mult, ALU.add, [bin_, tmn], [tmn])
                reduce(rq_, tm_, ALU.max, [tmn], ["rq"])
                tt(tm_, tm_, rq_.unsqueeze(2).to_broadcast([128, 4, NE]), ALU.is_equal, [tmn, "rq"], [tmn])
                tt(bi_, bi_, tm_, ALU.add, [bin_, tmn], [bin_])
                tt(bi_, bi_, sc_, ALU.mult, [bin_, scn], [bin_])
                reduce(rq_, bi_, ALU.add, [bin_], ["rq"])
                recip(rq_, "rq")
                tt(comb[:, tsl, :], bi_, rq_.unsqueeze(2).to_broadcast([128, 4, NE]), ALU.mult, [bin_, "rq"], ["comb%d" % tb])

            def gateup(e, tb, sGU, nGU, pre=None, post=None):
                for fc in range(4):
                    if pre is not None:
                        pre(fc)
                    fs = slice(fc * 128, (fc + 1) * 128)
                    gp, gn = PB()
                    mmk(gp[:], lambda k: sGU[:, k, fs], lambda k: bigB[:, tb, k, :], KC, [nGU[0]] + qn_[tb], [gn])
                    up, un = PB()
                    mmk(up[:], lambda k: sGU[:, k, 512 + fc * 128:512 + (fc + 1) * 128], lambda k: bigB[:, tb, k, :], KC,
                        [nGU[1]] + qn_[tb], [un])
                    slb, slname = (fA, "fA") if fc < 2 else (fB, "fB")
                    sls = slb[:, (fc % 2) * 512:(fc % 2 + 1) * 512]
                    act(sls, gp[:], AF.Silu, [gn], [slname])
                    tt(hid[:, fc, :], sls, up[:], ALU.mult, [slname, un], ["Sbf%d" % (fc // 2)])
                    if post is not None:
                        post(fc)

            def down(e, tb, sD, nD):
                for i in range(4):
                    t = 4 * tb + i
                    for half in range(2):
                        hs = slice(half * 512, (half + 1) * 512)
                        op_, on = PB()
                        mmk(op_[:], lambda k: hid[:, k, i * 128:(i + 1) * 128], lambda k: sD[:, k, hs], 4,
                            ["Sbf0", "Sbf1", nD[half]], [on])
                        stt(xres[:, t, hs], op_[:], comb[:, t, e:e + 1], xres[:, t, hs], ALU.mult, ALU.add,
                            [on, "comb%d" % tb, "x%d" % t], ["x%d" % t])

            for i in range(4):
                napply(0, i)
                ntrans(0, i)
            router(0)
            for e in range(NE):
                sGU, nGU = wload([wg_d[l, e].rearrange("(k p) n -> p k n", p=128), wu_d[l, e].rearrange("(k p) n -> p k n", p=128)])
                wdv = wd_d[l, e].rearrange("(k p) n -> p k n", p=128)
                sD, nD = wload([wdv[:, :, 0:512], wdv[:, :, 512:1024]], kdim=4)
                for hlf in range(2):
                    cs = slice(hlf * 512, (hlf + 1) * 512)
                    tt(sD[:, 0:4, cs], sD[:, 0:4, cs], mod[:, 2, cs].unsqueeze(1).to_broadcast([128, 4, 512]), ALU.mult,
                       [nD[hlf], "mod2"], [nD[hlf]])
                for tb in range(4):
                    if e == 0 and tb < 3:
                        gateup(e, tb, sGU, nGU, pre=lambda fc, tb=tb: napply(tb + 1, fc),
                               post=lambda fc, tb=tb: ntrans(tb + 1, fc))
                        router(tb + 1)
                    else:
                        gateup(e, tb, sGU, nGU)
                    down(e, tb, sD, nD)

        for l in range(n_layers):
            ada_part(l, 0)
            mixer(l)
            if l == n_layers - 1 and stop_after == "mixer":
                break
            ada_part(l, 1)
            moe(l)

        sdma(mod[:, 1, :], fing.partition_broadcast(128), [], ["mod1"])
        memset(stat[:, 32:48], 0.0, ["stat4"])
        for t in range(NT):
            act(junk_moe, xres[:, t, :], AF.Square, ["x%d" % t], ["Sbf1", "stat4"], accum=stat[:, 32 + t:33 + t])
        rstd_from(stat[:, 32:48], 1.0 / D, "stat4")
        okeys = []
        for t in range(NT):
            stt(xres[:, t, :], xres[:, t, :], stat[:, 32 + t:33 + t], mod[:, 1, :], ALU.mult, ALU.mult,
                ["x%d" % t, "stat4", "mod1"], ["x%d" % t])
            key = "dma:out%d" % (t % 4)
            sdma(out_d[t * 128:(t + 1) * 128, :], xres[:, t, :], ["x%d" % t], ["out%d" % t], key=key)
            if key not in okeys:
                okeys.append(key)
        S.emit()
        S.final_wait("sp", okeys)
    return nc


def _consts():
    ident = np.eye(128, dtype=np.float32)
    s = np.arange(128)[:, None]
    t = np.arange(128)[None, :]
    same = (s // 64) == (t // 64)
    tri = np.where(same & (s <= t), -1.0 / 16.0, 0.0).astype(np.float32)
    triu = np.where(same & (s > t), -1.0 / 16.0, 0.0).astype(np.float32)
    mask2 = np.where(same & (s <= t), 1.0, 0.0).astype(np.float32)
    cst = np.stack([tri, triu, mask2], axis=1).astype(np.float32)
    mprev = (s > t).astype(np.float32)
    mcur = (s <= t).astype(np.float32)
    msk = np.stack([mprev, mcur], axis=1).astype(np.float32)
    invf = np.power(np.float32(10000.0), -np.arange(32, dtype=np.float32) / np.float32(32)).astype(np.float32)
    invf = np.broadcast_to(invf[None, :], (128, 32)).copy()
    return cst, msk, invf, ident


_PROG_CACHE = {}


def _run(inputs, n_cores=8, n_layers=DEPTH, stop_after=None):
    key = (n_layers, stop_after)
    if key not in _PROG_CACHE:
        _PROG_CACHE[key] = build_program(n_layers, stop_after)
    nc = _PROG_CACHE[key]
    cst, msk, invf, ident = _consts()
    f32 = lambda a: np.ascontiguousarray(np.asarray(a), dtype=np.float32)
    shared = {k: f32(inputs[k]) for k in
              ["ada_w", "ada_b", "norm1_g", "norm2_g", "final_g", "w_in", "gla_alpha_w", "gla_alpha_b", "gla_norm_g",
               "swa_sinks", "w_pa", "w_pb", "w_out", "router_w", "router_b", "moe_w_gate", "moe_w_up", "moe_w_down"]}
    shared["cst_f32"] = cst
    shared["cst_msk"] = msk
    shared["cst_invf"] = invf
    shared["cst_ident"] = ident
    x = f32(inputs["x"])
    c = f32(inputs["c"])
    pos = np.ascontiguousarray(np.asarray(inputs["positions"]), dtype=np.int32)
    in_maps = []
    for b in range(n_cores):
        m = dict(shared)
        m["x"] = np.ascontiguousarray(x[b])
        m["c_col"] = np.ascontiguousarray(c[b].reshape(KC, 128).T)
        m["pos"] = np.ascontiguousarray(pos[b].reshape(NT, 128).T)
        in_maps.append(m)
    res = run_bass_kernel_spmd(nc, in_maps, core_ids=list(range(n_cores)))
    return np.stack([np.asarray(r["out"], dtype=np.float32) for r in res.results], axis=0)


def kernel(**inputs):
    return _run(inputs, n_cores=8, n_layers=DEPTH)
```
